# Optimizing a Trainium2 kernel written in Bass

```python
import jax, jax.numpy as jnp
from jax import lax
import numpy as np

D_MODEL = 4096
BATCH = 1
SEQ = 8192
DEPTH = 2

N_BRANCH = 3
RMS_EPS = 1e-6
LN_EPS = 1e-5
D_CONV = D_MODEL // 4
CONV_WIDTH = 31
ATT_HEADS = 8
ATT_HEAD_DIM = 128
D_ATT = ATT_HEADS * ATT_HEAD_DIM
ATT_SCALE = ATT_HEAD_DIM ** -0.5
IDX_HEADS = 16
IDX_HEAD_DIM = 64
IDX_SCALE = IDX_HEAD_DIM ** -0.5
IDX_W_SCALE = IDX_HEADS ** -0.5
TOPK_MAX = 256
Q_BLOCK = 128
ROPE_THETA = 500000.0
ROPE_FRACTION = 4
D_RWKV = D_MODEL // 2
RWKV_HEAD = 64
RWKV_HEADS = D_RWKV // RWKV_HEAD
LORA_DECAY = 96
LORA_AAA = 96
LORA_MV = 64
LORA_GATE = 256
RWKV_GN_EPS = 64e-5
D_FF = ((8 * D_MODEL + 3 * 256 - 1) // (3 * 256)) * 256

RWKV_SIZES = (D_RWKV, D_RWKV, D_RWKV, LORA_DECAY, LORA_AAA, LORA_GATE)
D_RWKV_IN = 3 * D_RWKV + LORA_DECAY + LORA_AAA + LORA_GATE
IDX_SIZES = (IDX_HEADS * IDX_HEAD_DIM, IDX_HEAD_DIM, IDX_HEADS)
D_IDX_IN = IDX_HEADS * IDX_HEAD_DIM + IDX_HEAD_DIM + IDX_HEADS
IN_SIZES = (2 * D_CONV, 3 * D_ATT, D_IDX_IN, D_RWKV_IN, N_BRANCH * D_MODEL)
D_IN = 2 * D_CONV + 3 * D_ATT + D_IDX_IN + D_RWKV_IN + N_BRANCH * D_MODEL

kernel_name = 'hybrid_conv_dsa_rwkv7_gated_block'


def split_cols(z, sizes):
    offs = np.cumsum(sizes)[:-1].tolist()
    return jnp.split(z, offs, axis=-1)


def rms_norm(x, g):
    xf = x.astype(jnp.float32)
    y = xf * lax.rsqrt(jnp.mean(xf * xf, axis=-1, keepdims=True) + RMS_EPS)
    return (y * g.astype(jnp.float32)).astype(x.dtype)


def layer_norm(x, g, b):
    xf = x.astype(jnp.float32)
    m = jnp.mean(xf, axis=-1, keepdims=True)
    var = jnp.mean(jnp.square(xf - m), axis=-1, keepdims=True)
    return ((xf - m) * lax.rsqrt(var + LN_EPS) * g + b).astype(x.dtype)


def partial_rope(x, pos):
    rd = x.shape[-1] // ROPE_FRACTION
    half = rd // 2
    inv_freq = jnp.power(ROPE_THETA, -jnp.arange(half, dtype=jnp.float32) * (2.0 / rd))
    ang = pos.astype(jnp.float32)[:, None] * inv_freq[None, :]
    cos = jnp.cos(ang)[None, :, None, :]
    sin = jnp.sin(ang)[None, :, None, :]
    x1 = x[..., :half].astype(jnp.float32)
    x2 = x[..., half:rd].astype(jnp.float32)
    rot = jnp.concatenate([x1 * cos - x2 * sin, x2 * cos + x1 * sin], axis=-1).astype(x.dtype)
    return jnp.concatenate([rot, x[..., rd:]], axis=-1)


def token_shift(z, mu):
    prev = jnp.pad(z, ((0, 0), (1, 0), (0, 0)))[:, :-1]
    return z + (prev - z) * mu


def conformer_conv(u, dw_w, dw_b, ln_g, ln_b):
    val, gate = jnp.split(u, 2, axis=-1)
    c = val * jax.nn.sigmoid(gate)
    c = lax.conv_general_dilated(
        c, dw_w[:, None, :], window_strides=(1,), padding=((CONV_WIDTH - 1, 0),),
        dimension_numbers=('NWC', 'WIO', 'NWC'), feature_group_count=D_CONV) + dw_b
    return jax.nn.silu(layer_norm(c, ln_g, ln_b))


def dsa_attention(q, k, v, iq, ik, iw):
    B, L = q.shape[0], q.shape[1]
    top_k = min(TOPK_MAX, L // 4)
    nb = L // Q_BLOCK
    key_pos = jnp.arange(L)
    q_pos = key_pos.reshape(nb, Q_BLOCK)

    def to_blocks(a):
        return jnp.moveaxis(a.reshape((B, nb, Q_BLOCK) + a.shape[2:]), 1, 0)

    gather = jax.vmap(lambda arr, idx: arr[idx])

    def block(args):
        qb, iqb, iwb, pos = args
        logits = jnp.einsum('bqhd,bsd->bqhs', iqb, ik, preferred_element_type=jnp.float32) * IDX_SCALE
        score = jnp.einsum('bqhs,bqh->bqs', jax.nn.relu(logits), iwb.astype(jnp.float32) * IDX_W_SCALE)
        causal = key_pos[None, :] <= pos[:, None]
        score = jnp.where(causal[None], score, -jnp.inf)
        _, idx = lax.top_k(score, top_k)
        valid = idx <= pos[None, :, None]
        ks = gather(k, idx)
        vs = gather(v, idx)
        s = jnp.einsum('bqhd,bqkhd->bhqk', qb, ks, preferred_element_type=jnp.float32) * ATT_SCALE
        s = jnp.where(valid[:, None], s, -jnp.inf)
        p = jax.nn.softmax(s, axis=-1).astype(vs.dtype)
        return jnp.einsum('bhqk,bqkhd->bqhd', p, vs)

    out = lax.map(block, (to_blocks(q), to_blocks(iq), to_blocks(iw), q_pos))
    return jnp.moveaxis(out, 0, 1).reshape(B, L, -1)


def wkv7_scan(r, w, k, v, a, b):
    B, _, H, N = r.shape

    def step(S, inp):
        r_t, w_t, k_t, v_t, a_t, b_t = inp
        sa = jnp.einsum('bhij,bhj->bhi', S, a_t)
        S = S * w_t[:, :, None, :] + sa[..., None] * b_t[:, :, None, :] + v_t[..., None] * k_t[:, :, None, :]
        return S, jnp.einsum('bhij,bhj->bhi', S, r_t)

    xs = tuple(jnp.moveaxis(t, 1, 0) for t in (r, w, k, v, a, b))
    _, y = lax.scan(step, jnp.zeros((B, H, N, N), jnp.float32), xs)
    return jnp.moveaxis(y, 0, 1)


def rwkv7_time_mix(z, mu, w0, w2, a0, a2, g2, k_k, k_a, r_k, ln_g, ln_b, v_first, vres):
    B, L, _ = z.shape
    f32 = jnp.float32
    r, k, v, xw, xa, xg = split_cols(token_shift(z, mu), RWKV_SIZES)
    if vres is not None:
        xv, v_up, v_bias = vres
        v = v + (v_first - v) * jax.nn.sigmoid(v_bias + xv @ v_up)
    w_log = -jax.nn.softplus(-(w0 + jnp.tanh(xw) @ w2)) - 0.5
    decay = jnp.exp(-jnp.exp(w_log.astype(f32)))
    a_lr = jax.nn.sigmoid(a0 + xa @ a2)
    g = jax.nn.sigmoid(xg) @ g2

    def heads(t):
        return t.astype(f32).reshape(B, L, RWKV_HEADS, RWKV_HEAD)

    kk = heads(k * k_k)
    kk = kk / jnp.maximum(jnp.sqrt(jnp.sum(kk * kk, axis=-1, keepdims=True)), 1e-12)
    k = k * (1 + (a_lr - 1) * k_a)
    rh, kh, vh = heads(r), heads(k), heads(v)
    y = wkv7_scan(rh, heads(decay), kh, vh, -kk, kk * heads(a_lr))
    m = jnp.mean(y, axis=-1, keepdims=True)
    var = jnp.mean(jnp.square(y - m), axis=-1, keepdims=True)
    y = ((y - m) * lax.rsqrt(var + RWKV_GN_EPS)).reshape(B, L, D_RWKV) * ln_g + ln_b
    bonus = jnp.sum(rh * kh * r_k, axis=-1, keepdims=True) * vh
    y = (y + bonus.reshape(B, L, D_RWKV)).astype(z.dtype) * g
    return y, v


def setup_inputs(seed: int = 0) -> dict:
    key = jax.random.key(seed)
    keys = iter(jax.random.split(key, 40))

    def nrm(shape, scale):
        return jax.random.normal(next(keys), shape, jnp.float32) * scale

    def uni(shape):
        return jax.random.uniform(next(keys), shape, jnp.float32)

    return {
        'x': nrm((BATCH, SEQ, D_MODEL), 1.0),
        'w_in': nrm((DEPTH, D_MODEL, D_IN), D_MODEL ** -0.5),
        'norm_mix': 1.0 + nrm((DEPTH, D_MODEL), 0.05),
        'dw_weight': nrm((DEPTH, CONV_WIDTH, D_CONV), CONV_WIDTH ** -0.5),
        'dw_bias': nrm((DEPTH, D_CONV), 0.01),
        'conv_ln_g': 1.0 + nrm((DEPTH, D_CONV), 0.05),
        'conv_ln_b': nrm((DEPTH, D_CONV), 0.01),
        'w_conv_out': nrm((DEPTH, D_CONV, D_MODEL), D_CONV ** -0.5),
        'w_att_out': nrm((DEPTH, D_ATT, D_MODEL), D_ATT ** -0.5),
        'rwkv_mu': uni((DEPTH, D_RWKV_IN)),
        'rwkv_w0': -0.5 + nrm((DEPTH, D_RWKV), 0.5),
        'rwkv_w2': nrm((DEPTH, LORA_DECAY, D_RWKV), 0.1 * LORA_DECAY ** -0.5),
        'rwkv_a0': nrm((DEPTH, D_RWKV), 0.1),
        'rwkv_a2': nrm((DEPTH, LORA_AAA, D_RWKV), LORA_AAA ** -0.5),
        'rwkv_g2': nrm((DEPTH, LORA_GATE, D_RWKV), LORA_GATE ** -0.5),
        'rwkv_k_k': 0.85 + nrm((DEPTH, D_RWKV), 0.05),
        'rwkv_k_a': 1.0 + nrm((DEPTH, D_RWKV), 0.05),
        'rwkv_r_k': nrm((DEPTH, RWKV_HEADS, RWKV_HEAD), 0.1),
        'rwkv_ln_g': 1.0 + nrm((DEPTH, D_RWKV), 0.05),
        'rwkv_ln_b': nrm((DEPTH, D_RWKV), 0.01),
        'vres_down': nrm((DEPTH - 1, D_MODEL, LORA_MV), D_MODEL ** -0.5),
        'vres_mu': uni((DEPTH - 1, LORA_MV)),
        'vres_up': nrm((DEPTH - 1, LORA_MV, D_RWKV), LORA_MV ** -0.5),
        'vres_bias': nrm((DEPTH - 1, D_RWKV), 0.1),
        'w_rwkv_out': nrm((DEPTH, D_RWKV, D_MODEL), D_RWKV ** -0.5),
        'w_out': nrm((DEPTH, D_MODEL, D_MODEL), D_MODEL ** -0.5),
        'norm_ffn': 1.0 + nrm((DEPTH, D_MODEL), 0.05),
        'w_ffn_gate': nrm((DEPTH, D_MODEL, D_FF), D_MODEL ** -0.5),
        'w_ffn_up': nrm((DEPTH, D_MODEL, D_FF), D_MODEL ** -0.5),
        'w_ffn_down': nrm((DEPTH, D_FF, D_MODEL), D_FF ** -0.5),
        'norm_final': 1.0 + nrm((D_MODEL,), 0.05),
    }


def reference(x, w_in, norm_mix, dw_weight, dw_bias, conv_ln_g, conv_ln_b, w_conv_out, w_att_out,
              rwkv_mu, rwkv_w0, rwkv_w2, rwkv_a0, rwkv_a2, rwkv_g2, rwkv_k_k, rwkv_k_a, rwkv_r_k,
              rwkv_ln_g, rwkv_ln_b, vres_down, vres_mu, vres_up, vres_bias, w_rwkv_out, w_out,
              norm_ffn, w_ffn_gate, w_ffn_up, w_ffn_down, norm_final):
    B, L, _ = x.shape
    pos = jnp.arange(L)
    v_first = None
    for l in range(DEPTH):
        h = rms_norm(x, norm_mix[l])
        z = h @ w_in[l]
        z_conv, z_att, z_idx, z_rwkv, z_gate = split_cols(z, IN_SIZES)
        a_out = conformer_conv(z_conv, dw_weight[l], dw_bias[l], conv_ln_g[l], conv_ln_b[l]) @ w_conv_out[l]
        q, k, v = [t.reshape(B, L, ATT_HEADS, ATT_HEAD_DIM) for t in jnp.split(z_att, 3, axis=-1)]
        q = partial_rope(q, pos)
        k = partial_rope(k, pos)
        iq, ik, iw = split_cols(z_idx, IDX_SIZES)
        iq = partial_rope(iq.reshape(B, L, IDX_HEADS, IDX_HEAD_DIM), pos)
        ik = partial_rope(ik[:, :, None, :], pos)[:, :, 0, :]
        b_out = dsa_attention(q, k, v, iq, ik, iw) @ w_att_out[l]
        vres = None
        if l > 0:
            xv = token_shift(h @ vres_down[l - 1], vres_mu[l - 1])
            vres = (xv, vres_up[l - 1], vres_bias[l - 1])
        c_mix, v_rwkv = rwkv7_time_mix(z_rwkv, rwkv_mu[l], rwkv_w0[l], rwkv_w2[l], rwkv_a0[l], rwkv_a2[l],
                                       rwkv_g2[l], rwkv_k_k[l], rwkv_k_a[l], rwkv_r_k[l], rwkv_ln_g[l],
                                       rwkv_ln_b[l], v_first, vres)
        if l == 0:
            v_first = v_rwkv
        c_out = c_mix @ w_rwkv_out[l]
        gate = jax.nn.sigmoid(z_gate).reshape(B, L, N_BRANCH, D_MODEL)
        merged = gate[:, :, 0] * a_out + gate[:, :, 1] * b_out + gate[:, :, 2] * c_out
        x = x + merged @ w_out[l]
        h = rms_norm(x, norm_ffn[l])
        x = x + (jax.nn.silu(h @ w_ffn_gate[l]) * (h @ w_ffn_up[l])) @ w_ffn_down[l]
    return rms_norm(x, norm_final)
```

```python
import numpy as np
import ml_dtypes
import concourse.bass as bass
import concourse.mybir as mybir
from concourse.bass_utils import run_bass_kernel_spmd
from contextlib import ExitStack

F32 = mybir.dt.float32
BF16 = mybir.dt.bfloat16
ALU = mybir.AluOpType
AF = mybir.ActivationFunctionType
AX = mybir.AxisListType


class V:
    __slots__ = ("b", "ap")

    def __init__(self, b, ap):
        self.b = b
        self.ap = ap

    def __getitem__(self, idx):
        return V(self.b, self.ap[idx])

    def r(self, pat, **kw):
        return V(self.b, self.ap.rearrange(pat, **kw))

    def bc(self, shape):
        return V(self.b, self.ap.broadcast_to(shape))


class Buf:
    def __init__(self, S, t, name):
        self.S = S
        self.t = t
        self.name = name
        self.lw = {}
        self.rd = {}

    def __getitem__(self, idx):
        return V(self, self.t[idx])

    @property
    def v(self):
        return V(self, self.t[:])

    def sub(self, ap, name=None):
        return Buf(self.S, ap, name or self.name + "_sub")


class EngState:
    def __init__(self, name, e, sem):
        self.name = name
        self.e = e
        self.sem = sem
        self.seq = 0
        self.known = {}
        self.snap = {}
        self._snapref = None
        self._dirty = True


class Sched:
    NDMA = 12

    def __init__(self, nc, es):
        self.nc = nc
        self.es = es
        self.eng = {}
        self.sems = {}
        for name, e in (("pe", nc.tensor), ("act", nc.scalar), ("dve", nc.vector),
                        ("pool", nc.gpsimd), ("sp", nc.sync)):
            sem = es.enter_context(nc.semaphore("s_" + name))
            self.eng[name] = EngState(name, e, sem)
            self.sems[name] = sem
        self.dma_sems = {}
        self.dma_cnt = {}
        self.dma_rr = {}
        for q in ("sp", "act", "pool"):
            lst = []
            for i in range(self.NDMA):
                key = "d_%s%d" % (q, i)
                sem = es.enter_context(nc.semaphore(key))
                self.sems[key] = sem
                self.dma_cnt[key] = 0
                lst.append(key)
            self.dma_sems[q] = lst
            self.dma_rr[q] = 0
        self.dma_snap = {}
        self.n_wait = 0
        self.n_ins = 0

    def sb(self, name, shape, dt, es=None):
        self._uid = getattr(self, "_uid", 0) + 1
        name = "%s_u%d" % (name, self._uid)
        t = (es or self.es).enter_context(self.nc.sbuf_tensor(name, list(shape), dt))
        return Buf(self, t, name)

    def ps(self, name, shape, dt=F32, es=None):
        t = (es or self.es).enter_context(self.nc.psum_tensor(name, list(shape), dt))
        return Buf(self, t, name)

    def dram(self, name, shape, dt, kind="Internal", **kw):
        t = self.nc.dram_tensor(name, list(shape), dt, kind=kind, **kw)
        b = Buf(self, t.ap(), name)
        return b

    def _need(self, X, key, val):
        st = self.eng[X]
        if st.known.get(key, 0) >= val:
            return
        st.e.wait_ge(self.sems[key], val)
        self.n_wait += 1
        st.known[key] = val
        st._dirty = True
        snap = None
        if key in self.eng:
            snap = self.eng[key].snap.get(val)
        else:
            snap = self.dma_snap.get((key, val))
        if snap:
            for k, v in snap.items():
                if st.known.get(k, 0) < v:
                    st.known[k] = v

    def _deps(self, X, R, W, is_dma=False):
        for v in R:
            b = v.b if isinstance(v, V) else v
            for k, val in b.lw.items():
                if k == X and not is_dma:
                    if X != "pe":
                        self._need(X, k, val)
                else:
                    self._need(X, k, val)
        for v in W:
            b = v.b if isinstance(v, V) else v
            for k, val in b.lw.items():
                if k != X or is_dma or X != "pe":
                    self._need(X, k, val)
            for k, val in b.rd.items():
                if k != X or is_dma or X != "pe":
                    self._need(X, k, val)

    def _snapshot(self, st):
        if st._dirty or st._snapref is None:
            st._snapref = dict(st.known)
            st._dirty = False
        return st._snapref

    def _commit(self, X, ins, R, W):
        st = self.eng[X]
        st.seq += 1
        ins.then_inc(st.sem, 1)
        self.n_ins += 1
        sn = self._snapshot(st)
        st.snap[st.seq] = sn
        tok = (X, st.seq)
        for v in R:
            b = v.b if isinstance(v, V) else v
            b.rd[X] = st.seq
        for v in W:
            b = v.b if isinstance(v, V) else v
            b.lw[X] = st.seq
        return tok

    def op(self, X, fn, R, W, *a, **kw):
        self._deps(X, R, W)
        ins = fn(*a, **kw)
        return self._commit(X, ins, R, W)

    def dma(self, q, out, in_, **kw):
        self._deps(q, [in_], [out], is_dma=True)
        st = self.eng[q]
        lst = self.dma_sems[q]
        key = lst[self.dma_rr[q] % len(lst)]
        self.dma_rr[q] += 1
        if self.dma_cnt[key]:
            self._need(q, key, self.dma_cnt[key])
        self.dma_cnt[key] += 16
        val = self.dma_cnt[key]
        ins = st.e.dma_start(out=out.ap, in_=in_.ap, **kw)
        ins.then_inc(self.sems[key], 16)
        self.n_ins += 1
        tok = (key, val)
        self.dma_snap[tok] = self._snapshot(st)
        in_.b.rd[key] = val
        out.b.lw[key] = val
        return tok

    def wait_tok(self, X, tok):
        self._need(X, tok[0], tok[1])

    def barrier_all(self):
        finals = {}
        for name, st in self.eng.items():
            if st.seq:
                finals[name] = st.seq
        for key, cnt in self.dma_cnt.items():
            if cnt:
                finals[key] = cnt
        for X in self.eng:
            for k, v in finals.items():
                if k != X:
                    self._need(X, k, v)

    def mm(self, out, lhsT, rhs, start=True, stop=True, **kw):
        return self.op("pe", self.nc.tensor.matmul, [lhsT, rhs], [out],
                       out.ap, lhsT.ap, rhs.ap, start=start, stop=stop, **kw)

    def tr(self, out, in_, ident):
        return self.op("pe", self.nc.tensor.transpose, [in_, ident], [out],
                       out.ap, in_.ap, ident.ap)

    def _e(self, X):
        return self.eng[X].e

    def act(self, out, in_, func, bias=None, scale=None, accum=None, X="act"):
        R = [in_]
        kw = {}
        if bias is not None:
            if isinstance(bias, V):
                R.append(bias); kw["bias"] = bias.ap
            else:
                kw["bias"] = bias
        if scale is not None:
            if isinstance(scale, V):
                R.append(scale); kw["scale"] = scale.ap
            else:
                kw["scale"] = scale
        W = [out]
        if accum is not None:
            W.append(accum); kw["accum_out"] = accum.ap
        return self.op("act", self.nc.scalar.activation, R, W, out.ap, in_.ap, func, **kw)

    def tt(self, X, out, a, b, op):
        return self.op(X, self._e(X).tensor_tensor, [a, b], [out], out.ap, a.ap, b.ap, op)

    def ts(self, X, out, a, s1, s2, op0, op1=None, accum=None):
        R = [a]
        if isinstance(s1, V):
            R.append(s1); s1 = s1.ap
        if isinstance(s2, V):
            R.append(s2); s2 = s2.ap
        W = [out]
        kw = {}
        if accum is not None:
            W.append(accum); kw["accum_out"] = accum.ap
        if op1 is None:
            return self.op(X, self._e(X).tensor_scalar, R, W, out.ap, a.ap, s1, None, op0, **kw)
        return self.op(X, self._e(X).tensor_scalar, R, W, out.ap, a.ap, s1, s2, op0, op1, **kw)

    def stt(self, X, out, a, s, b, op0, op1, accum=None):
        R = [a, b]
        if isinstance(s, V):
            R.append(s); s = s.ap
        W = [out]
        kw = {}
        if accum is not None:
            W.append(accum); kw["accum_out"] = accum.ap
        return self.op(X, self._e(X).scalar_tensor_tensor, R, W, out.ap, a.ap, s, b.ap, op0, op1, **kw)

    def cp(self, X, out, in_):
        if X == "act":
            return self.op("act", self.nc.scalar.copy, [in_], [out], out.ap, in_.ap)
        return self.op(X, self._e(X).tensor_copy, [in_], [out], out.ap, in_.ap)

    def memset(self, X, out, val):
        return self.op(X, self._e(X).memset, [], [out], out.ap, val)

    def red(self, X, out, in_, op, axis=AX.X):
        return self.op(X, self._e(X).tensor_reduce, [in_], [out], out.ap, in_.ap, axis, op)

    def recip(self, out, in_):
        return self.op("dve", self.nc.vector.reciprocal, [in_], [out], out.ap, in_.ap)

    def allgather(self, pairs):
        nc = self.nc
        if not hasattr(self, "ccsem"):
            self.ccsem = self.es.enter_context(nc.semaphore("ccsem"))
            self.cc_cnt = 0
        self.barrier_all()
        nc.all_engine_barrier()
        for (i, o) in pairs:
            self.cc_cnt += 1
            nc.gpsimd.collective_compute(
                "AllGather", ALU.bypass, replica_groups=[list(range(8))],
                ins=[i.t], outs=[o.t]).then_inc(self.ccsem, 1)
            nc.gpsimd.wait_ge(self.ccsem, self.cc_cnt)
        for X, st in self.eng.items():
            if X != "pool":
                st.e.wait_ge(self.ccsem, self.cc_cnt)

ROPE_THETA = 500000.0
RMS_EPS = 1e-6
LN_EPS = 1e-5
GN_EPS = 64e-5
NEG = -30000.0


class Cfg:
    def __init__(self, D=4096, L=8192, depth=2):
        self.D = D; self.L = L; self.depth = depth; self.NC = 8
        self.TC = L // 8; self.NT = self.TC // 128
        self.TB = min(512, self.TC); self.NTB = self.TC // self.TB
        self.KD = D // 128
        self.DC = D // 4; self.NCV = self.DC // 128
        self.DR = D // 2; self.HR = self.DR // 64; self.NP = self.DR // 128
        self.DFF = ((8 * D + 3 * 256 - 1) // (3 * 256)) * 256; self.KF = self.DFF // 128
        self.DRIN = 3 * self.DR + 96 + 96 + 256
        self.DIDX = 1024 + 64 + 16
        self.DIN = 2 * self.DC + 3072 + self.DIDX + self.DRIN + 3 * D
        o = 0
        self.o_conv = o; o += 2 * self.DC
        self.o_q = o; self.o_k = o + 1024; self.o_v = o + 2048; o += 3072
        self.o_iq = o; self.o_ik = o + 1024; self.o_iw = o + 1088; o += self.DIDX
        self.o_rw = o; o += self.DRIN
        self.o_gate = o; o += 3 * D
        assert o == self.DIN
        self.topk = min(256, L // 4)
        self.NZR = (self.DRIN + 127) // 128
        self.NTL = 32 * self.NCV + self.NZR + 1


def make_consts(cfg, core):
    TC, L = cfg.TC, cfg.L
    pos = np.arange(core * TC, (core + 1) * TC, dtype=np.float64)
    c = {}
    c["ident"] = np.eye(128, dtype=np.float32)
    cosA = np.ones((128, TC)); sinA = np.zeros((128, TC))
    inv = ROPE_THETA ** (-np.arange(16) * (2.0 / 32))
    ang = inv[:, None] * pos[None, :]
    cosA[0:16] = np.cos(ang); cosA[16:32] = np.cos(ang)
    sinA[0:16] = np.sin(ang); sinA[16:32] = np.sin(ang)
    PA = np.zeros((128, 128))
    for m in range(16):
        PA[m, m + 16] = -1.0
        PA[m + 16, m] = 1.0
    cosI = np.ones((128, TC)); sinI = np.zeros((128, TC))
    inv = ROPE_THETA ** (-np.arange(8) * (2.0 / 16))
    ang = inv[:, None] * pos[None, :]
    PI = np.zeros((128, 128))
    for b in (0, 64):
        cosI[b:b + 8] = np.cos(ang); cosI[b + 8:b + 16] = np.cos(ang)
        sinI[b:b + 8] = np.sin(ang); sinI[b + 8:b + 16] = np.sin(ang)
        for m in range(8):
            PI[b + m, b + m + 8] = -1.0
            PI[b + m + 8, b + m] = 1.0
    c["rope"] = np.stack([cosA, sinA, cosI, sinI]).astype(np.float32)
    c["ropeP"] = np.stack([PA.T, PI.T]).astype(np.float32)
    qpos = pos.reshape(cfg.NT, 128).T
    c["qpos"] = np.ascontiguousarray(qpos).astype(np.float32)
    c["sidx"] = np.arange(L, dtype=np.float32)[None, :]
    sel = np.zeros((128, 8), np.float32)
    if core > 0:
        sel[:, core - 1] = 1.0
    c["selprev"] = sel
    selpre = np.zeros((128, 8), np.float32)
    selpre[:, core] = 1.0
    c["selpre"] = selpre
    s = np.arange(128)[:, None]; t = np.arange(128)[None, :]
    same = (s // 64) == (t // 64)
    mSU = (same & (s < t)); mSL = (same & (s > t)); mU = (same & (s <= t))
    c["bmask"] = np.stack([mSU, mSL, mU, same]).astype(np.float32)
    rp = np.ones((128, TC), np.float32)
    rp[:, 0::64] = 0.0
    c["scanrst"] = rp
    return c


CONST_SHAPES = lambda cfg: {
    "ident": [128, 128], "rope": [4, 128, cfg.TC], "ropeP": [2, 128, 128], "qpos": [128, cfg.NT],
    "sidx": [1, cfg.L], "selprev": [128, 8], "selpre": [128, 8], "bmask": [4, 128, 128],
    "scanrst": [128, cfg.TC],
}

PARAM_NAMES = ['w_in', 'norm_mix', 'dw_weight', 'dw_bias', 'conv_ln_g', 'conv_ln_b', 'w_conv_out', 'w_att_out',
               'rwkv_mu', 'rwkv_w0', 'rwkv_w2', 'rwkv_a0', 'rwkv_a2', 'rwkv_g2', 'rwkv_k_k', 'rwkv_k_a', 'rwkv_r_k',
               'rwkv_ln_g', 'rwkv_ln_b', 'vres_down', 'vres_mu', 'vres_up', 'vres_bias', 'w_rwkv_out', 'w_out',
               'norm_ffn', 'w_ffn_gate', 'w_ffn_up', 'w_ffn_down', 'norm_final']


SPLIT_ELEMS = 2 * 1024 * 1024


def split_cols(rows, cols):
    if rows * cols <= SPLIT_ELEMS:
        return None
    pw = max(256, (SPLIT_ELEMS // rows) // 256 * 256)
    return [(c, min(cols, c + pw)) for c in range(0, cols, pw)]


class Pieces:
    def __init__(self, items):
        self.items = items


class SW:
    def __init__(self, aps, ranges):
        self.aps = aps
        self.ranges = ranges

    def __getitem__(self, idx):
        rs, cs = idx
        c0 = cs.start or 0
        c1 = cs.stop
        out = []
        for ap, (a, b) in zip(self.aps, self.ranges):
            lo, hi = max(a, c0), min(b, c1)
            if lo < hi:
                out.append((ap[rs, lo - a:hi - a], lo - c0))
        return Pieces(out)


class LP:
    def __init__(self, ap, layer):
        self.ap = ap
        self.layer = layer

    def __getitem__(self, idx):
        if isinstance(idx, tuple):
            assert idx[0] == self.layer, (idx, self.layer)
            rest = idx[1:]
            return self.ap[rest[0]] if len(rest) == 1 else self.ap[rest]
        assert idx == self.layer, (idx, self.layer)
        return self.ap


class K:
    def __init__(self, cfg, dbg=(), ext_in=(), ext_out=(), params=None, use_x=True, use_out=True):
        self.cfg = cfg
        self.dbg = set(dbg)
        self.ext_in = set(ext_in)
        self.ext_out = set(ext_out)
        self.params = params
        self.use_x = use_x
        self.use_out = use_out
        self.nc = bass.Bass("TRN2", target_bir_lowering=False)
        self.es = ExitStack()
        self.outs = []

    def scratch(self, name, shape, dt):
        kind = "Internal"
        if name in self.ext_in:
            kind = "ExternalInput"
        elif name in self.ext_out:
            kind = "ExternalOutput"
        b = self.S.dram(name, shape, dt, kind=kind)
        if name in self.dbg:
            o = self.S.dram("dbg_" + name, shape, dt, kind="ExternalOutput")
            self.outs.append((b, o))
        return b

    def flush_dbg(self):
        for (b, o) in self.outs:
            self.S.dma("sp", o.v, b.v)
        self.S.barrier_all()

    def psum_bank(self):
        b = self.banks[self.bank_rr % 8]
        self.bank_rr += 1
        return b

    def colvec(self, src_ap_1d, n, name, es, dt=F32):
        S = self.S
        if n >= 128:
            k = n // 128
            t = S.sb(name, [128, k], dt, es=es)
            S.dma("sp", t.v, V(self.pbuf, src_ap_1d.rearrange("(k p) -> p k", p=128)), allow_slow_non_contiguous=True)
        else:
            t = S.sb(name, [n, 1], dt, es=es)
            S.dma("sp", t.v, V(self.pbuf, src_ap_1d.rearrange("(p o) -> p o", o=1)), allow_slow_non_contiguous=True)
        return t

def param_shapes(cfg):
    D = cfg.D; dp = cfg.depth
    return {
        'w_in': [dp, D, cfg.DIN], 'norm_mix': [dp, D], 'dw_weight': [dp, 31, cfg.DC], 'dw_bias': [dp, cfg.DC],
        'conv_ln_g': [dp, cfg.DC], 'conv_ln_b': [dp, cfg.DC], 'w_conv_out': [dp, cfg.DC, D],
        'w_att_out': [dp, 1024, D], 'rwkv_mu': [dp, cfg.DRIN], 'rwkv_w0': [dp, cfg.DR],
        'rwkv_w2': [dp, 96, cfg.DR], 'rwkv_a0': [dp, cfg.DR], 'rwkv_a2': [dp, 96, cfg.DR],
        'rwkv_g2': [dp, 256, cfg.DR], 'rwkv_k_k': [dp, cfg.DR], 'rwkv_k_a': [dp, cfg.DR],
        'rwkv_r_k': [dp, cfg.HR, 64], 'rwkv_ln_g': [dp, cfg.DR], 'rwkv_ln_b': [dp, cfg.DR],
        'vres_down': [dp - 1, D, 64], 'vres_mu': [dp - 1, 64], 'vres_up': [dp - 1, 64, cfg.DR],
        'vres_bias': [dp - 1, cfg.DR], 'w_rwkv_out': [dp, cfg.DR, D], 'w_out': [dp, D, D],
        'norm_ffn': [dp, D], 'w_ffn_gate': [dp, D, cfg.DFF], 'w_ffn_up': [dp, D, cfg.DFF],
        'w_ffn_down': [dp, cfg.DFF, D], 'norm_final': [D],
    }


def k_setup(self):
    nc, cfg = self.nc, self.cfg
    S = self.S = Sched(nc, self.es)
    self.pbuf = Buf(S, None, "params")
    if self.use_x:
        self.x_in = nc.dram_tensor("x", [cfg.TC, cfg.D], F32, kind="ExternalInput").ap()
    self.P = {}
    self.split_info = {}
    shp = param_shapes(cfg)
    if self.params is None:
        for n in PARAM_NAMES:
            self.P[n] = nc.dram_tensor(n, shp[n], F32, kind="ExternalInput").ap()
    else:
        for n, li in self.params.items():
            if n == 'norm_final':
                self.P[n] = nc.dram_tensor(n, shp[n], F32, kind="ExternalInput").ap()
            else:
                sh = shp[n][1:]
                rng_ = split_cols(sh[0], sh[1]) if len(sh) == 2 else None
                if rng_ is None:
                    self.P[n] = LP(nc.dram_tensor(n, sh, F32, kind="ExternalInput").ap(), li)
                else:
                    aps = [nc.dram_tensor("%s__%d" % (n, i), [sh[0], b - a], F32, kind="ExternalInput").ap()
                           for i, (a, b) in enumerate(rng_)]
                    self.P[n] = LP(SW(aps, rng_), li)
                    self.split_info[n] = rng_
    self.C = {}
    for n, s in CONST_SHAPES(cfg).items():
        self.C[n] = nc.dram_tensor("c_" + n, s, F32, kind="ExternalInput").ap()
    if self.use_out:
        self.out = S.dram("out", [cfg.TC, cfg.D], F32, kind="ExternalOutput")
    self.banks = [S.ps("bank%d" % i, [128, 512], F32) for i in range(8)]
    self.bank_rr = 0
    es = self.es
    self.identf = S.sb("identf", [128, 128], F32)
    S.dma("sp", self.identf.v, V(self.pbuf, self.C["ident"]))
    self.identb = S.sb("identb", [128, 128], BF16)
    S.cp("dve", self.identb.v, self.identf.v)
    self.onesb = S.sb("onesb", [128, 128], BF16)
    S.memset("dve", self.onesb.v, 1.0)
    self.xT = self.scratch("xT", [cfg.D, cfg.TC], F32)
    self.xT_src = self.scratch("xT_in", [cfg.D, cfg.TC], F32) if "xT_in" in self.ext_in else self.xT


def k_load_x(self):
    S, cfg = self.S, self.cfg
    with ExitStack() as es:
        xin = [S.sb("xin%d" % i, [128, cfg.D], F32, es=es) for i in range(2)]
        stg = [S.sb("xstg%d" % i, [128, 4, 128], F32, es=es) for i in range(2)]
        n = 0
        for m in range(cfg.NT):
            xt = xin[m % 2]
            S.dma("sp", xt.v, V(self.pbuf, self.x_in[m * 128:(m + 1) * 128, :]))
            for k4 in range(cfg.KD // 4):
                bank = self.psum_bank()
                for j in range(4):
                    k = k4 * 4 + j
                    S.tr(bank[:, j * 128:(j + 1) * 128], xt[:, k * 128:(k + 1) * 128], self.identf.v)
                st = stg[n % 2]; n += 1
                S.cp("act" if n % 2 else "dve", st.v.r("p a b -> p (a b)"), bank.v)
                dst = self.xT.t[k4 * 512:(k4 + 1) * 512, m * 128:(m + 1) * 128].rearrange("(a p) t -> p a t", p=128)
                S.dma("sp", V(self.xT, dst), st.v)
        S.barrier_all()


def k_norm(self, gvec_ap, hT, es_outer):
    S, cfg = self.S, self.cfg
    with ExitStack() as es:
        gcol = k_colparams(self, [gvec_ap], cfg.D, "gcol", es)
        xblk = S.sb("xblk", [128, cfg.KD, cfg.TB], F32, es=es)
        sq = [S.sb("sq%d" % i, [128, cfg.TB], BF16, es=es) for i in range(2)]
        rstd = S.sb("rstd", [128, cfg.TB], F32, es=es)
        for tb in range(cfg.NTB):
            t0 = tb * cfg.TB
            acc = self.psum_bank()
            for k in range(cfg.KD):
                S.dma("sp", xblk[:, k, :], self.xT[k * 128:(k + 1) * 128, t0:t0 + cfg.TB])
                s = sq[k % 2]
                S.act(s.v, xblk[:, k, :], AF.Square)
                S.mm(acc[:, 0:cfg.TB], self.onesb.v, s.v, start=(k == 0), stop=(k == cfg.KD - 1))
            S.ts("dve", rstd.v, acc[:, 0:cfg.TB], 1.0 / cfg.D, RMS_EPS, ALU.mult, ALU.add)
            S.act(rstd.v, rstd.v, AF.Sqrt)
            S.recip(rstd.v, rstd.v)
            for k in range(cfg.KD):
                S.stt("dve", hT[:, k, t0:t0 + cfg.TB], xblk[:, k, :], gcol[:, k, 0:1], rstd.v, ALU.mult, ALU.mult)
        S.barrier_all()


class WStream:
    def __init__(self, kb, KC, es, nbuf=2, width=512, name="wb"):
        self.kb = kb
        self.KC = KC
        self.bufs = [kb.S.sb("%s%d" % (name, i), [128, KC, width], BF16, es=es) for i in range(nbuf)]
        self.rr = 0
        import os as _os
        self.castdma = getattr(kb, "castdma", True) and not _os.environ.get("NOCAST")
        self.nst = 0
        if not self.castdma:
            self.stg = [kb.S.sb("%sst%d" % (name, i), [128, min(4, KC), width], F32, es=es) for i in range(2)]

    def load(self, pieces):
        S = self.kb.S
        wb = self.bufs[self.rr % len(self.bufs)]
        self.rr += 1
        kstep = 4 if self.KC >= 4 else self.KC
        flat = []
        for (ap, off) in pieces:
            if isinstance(ap, Pieces):
                flat += [(a, off + rel) for (a, rel) in ap.items]
            else:
                flat.append((ap, off))
        for (ap, off) in flat:
            w = ap.shape[1]
            import os as _os
            _ksel = _os.environ.get("KSEL")
            for k0 in range(0, self.KC, kstep):
                if _ksel is not None and str(k0 // kstep) not in _ksel:
                    continue
                k1 = min(self.KC, k0 + kstep)
                src = ap[k0 * 128:k1 * 128, :].rearrange("(k p) c -> p k c", p=128)
                if self.castdma:
                    S.dma("pool", wb[:, k0:k1, off:off + w], V(self.kb.pbuf, src))
                else:
                    st = self.stg[self.nst % 2]
                    self.nst += 1
                    S.dma("sp", st[:, 0:k1 - k0, 0:w], V(self.kb.pbuf, src))
                    S.cp("dve" if self.nst % 2 else "act", wb[:, k0:k1, off:off + w], st[:, 0:k1 - k0, 0:w])
        return wb


def gemm_fm(self, wb, ncols, KC, act_k, consumer, col0=0):
    S, cfg = self.S, self.cfg
    j = 0
    c = 0
    while c < ncols:
        w = min(128, ncols - c)
        pss = [self.psum_bank() for _ in range(cfg.NTB)]
        for k in range(KC):
            for tb in range(cfg.NTB):
                S.mm(pss[tb][0:w, 0:cfg.TB], wb[:, k, col0 + c:col0 + c + w],
                     act_k(k)[:, tb * cfg.TB:(tb + 1) * cfg.TB], start=(k == 0), stop=(k == KC - 1))
        consumer(j, w, [p[0:w, 0:cfg.TB] for p in pss])
        c += w
        j += 1


def gemm_tm(self, wb, ncols, KC, act_k, consumer, col0=0):
    S, cfg = self.S, self.cfg
    for m in range(cfg.NT):
        ps = self.psum_bank()
        for k in range(KC):
            S.mm(ps[:, 0:ncols], act_k(k)[:, m * 128:(m + 1) * 128], wb[:, k, col0:col0 + ncols],
                 start=(k == 0), stop=(k == KC - 1))
        consumer(m, ps[:, 0:ncols])

def k_alloc_scratch(self):
    cfg = self.cfg
    sc = self.scratch
    TC = cfg.TC
    self.cT = sc("cT", [cfg.DC, TC], F32)
    self.qT = sc("qT", [1024, TC], BF16)
    self.kloc = sc("kloc", [1024, TC], BF16)
    self.kall = sc("kall", [8 * 1024, TC], BF16)
    self.vloc = sc("vloc", [TC, 1024], BF16)
    self.vall = sc("vall", [8 * TC, 1024], BF16)
    self.iqT = sc("iqT", [1024, TC], BF16)
    self.ikloc = sc("ikloc", [64, TC], BF16)
    self.ikall = sc("ikall", [8 * 64, TC], BF16)
    self.iw = sc("iw", [TC, 16], F32)
    self.zr = sc("zr", [cfg.DRIN, TC], F32)
    self.xvp = sc("xvp", [64, TC], F32)
    self.gateT = [sc("gateT%d" % i, [cfg.D, TC], F32) for i in range(3)]
    self.gateT_src = ([sc("gateT_in%d" % i, [cfg.D, TC], F32) for i in range(3)]
                      if "gateT_in0" in self.ext_in else self.gateT)
    self.tails_loc = sc("tails_loc", [128, cfg.NTL], F32)
    self.tails_all = sc("tails_all", [8 * 128, cfg.NTL], F32)


def k_phaseA(self, l):
    S, cfg, P = self.S, self.cfg, self.P
    TB, NTB, KD = cfg.TB, cfg.NTB, cfg.KD
    with ExitStack() as es:
        import os as _os
        ACUT = int(_os.environ.get("ACUT", "99"))
        if ACUT < 1:
            return
        hT = S.sb("hT", [128, KD, cfg.TC], BF16, es=es)
        k_norm(self, P['norm_mix'][l], hT, es)
        if ACUT < 2:
            S.barrier_all()
            return
        act_k = lambda k: hT[:, k, :]
        ws = WStream(self, KD, es)
        _sk = _os.environ.get("ASKIP", "")
        rope = S.sb("ropeT", [128, 4, cfg.TC], F32, es=es)
        if "r" not in _sk:
            S.dma("sp", rope.v, V(self.pbuf, self.C["rope"].rearrange("a p t -> p a t")))
        rPf = S.sb("rPf", [128, 2, 128], F32, es=es)
        rP = S.sb("rP", [128, 2, 128], BF16, es=es)
        if "p" not in _sk:
            S.dma("sp", rPf.v, V(self.pbuf, self.C["ropeP"].rearrange("a p t -> p a t")))
            S.cp("dve", rP.v, rPf.v)
        tails = S.sb("tails", [128, cfg.NTL], F32, es=es)
        if "t" not in _sk:
            S.memset("dve", tails.v, 0.0)
        f32s = [S.sb("stf%d" % i, [128, 512], F32, es=es) for i in range(6)]
        bfs = [S.sb("stb%d" % i, [128, 512], BF16, es=es) for i in range(4)]
        cnt = {"f": 0, "b": 0, "e": 0}

        def stf():
            cnt["f"] += 1
            return f32s[cnt["f"] % 6]

        def stb():
            cnt["b"] += 1
            return bfs[cnt["b"] % 4]

        def alt():
            cnt["e"] += 1
            return "act" if cnt["e"] % 2 else "dve"

        W = P['w_in'][l]

        VW = min(int(_os.environ.get('VWMAX', '256')), cfg.DC)
        for j0 in range(0, cfg.DC, VW):
            wb = ws.load([(W[:, cfg.o_conv + j0:cfg.o_conv + j0 + VW], 0),
                          (W[:, cfg.o_conv + cfg.DC + j0:cfg.o_conv + cfg.DC + j0 + VW], VW)])
            held = {}
            GKC = int(_os.environ.get("GKC", str(KD)))
            if GKC == 0:
                continue
            gemm_fm(self, wb, VW, GKC, act_k, lambda j, w, pss: held.__setitem__(j, pss), col0=0)

            def cons_gate(j, w, pss, j0=j0, held=held):
                ch = j0 // 128 + j
                for tb in range(NTB):
                    sg = stf()
                    S.act(sg[:, 0:TB], pss[tb], AF.Sigmoid)
                    cs = stf()
                    S.tt("dve", cs[:, 0:TB], sg[:, 0:TB], held[j][tb], ALU.mult)
                    S.dma("sp", self.cT[ch * 128:(ch + 1) * 128, tb * TB:(tb + 1) * TB], cs[:, 0:TB])
                    if tb == NTB - 1:
                        S.cp("act", tails[:, ch * 32:(ch + 1) * 32], cs[:, TB - 32:TB])
            gemm_fm(self, wb, VW, GKC, act_k, cons_gate, col0=VW)

        if ACUT < 3:
            S.barrier_all()
            return
        def rope_cons(kind, dest, row0):
            def cons(j, w, pss):
                for tb in range(NTB):
                    tsl = slice(tb * TB, (tb + 1) * TB)
                    zf = stf()
                    S.cp("act", zf[0:w, 0:TB], pss[tb])
                    zb = stb()
                    S.cp("dve", zb[0:w, 0:TB], zf[0:w, 0:TB])
                    t1 = stf()
                    S.tt("dve", t1[0:w, 0:TB], zf[0:w, 0:TB], rope[0:w, 2 * kind, tsl], ALU.mult)
                    pz = self.psum_bank()
                    S.mm(pz[0:w, 0:TB], rP[0:w, kind, 0:w], zb[0:w, 0:TB])
                    t2 = stf()
                    S.tt("dve", t2[0:w, 0:TB], pz[0:w, 0:TB], rope[0:w, 2 * kind + 1, tsl], ALU.mult)
                    ob = stb()
                    S.tt("dve", ob[0:w, 0:TB], t2[0:w, 0:TB], t1[0:w, 0:TB], ALU.add)
                    S.dma("sp", dest[row0 + j * 128:row0 + j * 128 + w, tsl], ob[0:w, 0:TB])
            return cons

        for (off, kind, dest) in ((cfg.o_q, 0, self.qT), (cfg.o_k, 0, self.kloc), (cfg.o_iq, 1, self.iqT)):
            for b in range(2):
                wb = ws.load([(W[:, off + b * 512:off + (b + 1) * 512], 0)])
                gemm_fm(self, wb, 512, KD, act_k, rope_cons(kind, dest, b * 512))

        if ACUT < 4:
            S.barrier_all()
            return
        wb = ws.load([(W[:, cfg.o_ik:cfg.o_ik + 80], 0)])
        gemm_fm(self, wb, 64, KD, act_k, rope_cons(1, self.ikloc, 0))

        def cons_iw(m, ps):
            st = stf()
            S.cp(alt(), st[:, 0:16], ps)
            S.dma("sp", self.iw[m * 128:(m + 1) * 128, :], st[:, 0:16])
        gemm_tm(self, wb, 16, KD, act_k, cons_iw, col0=64)

        for b in range(2):
            wb = ws.load([(W[:, cfg.o_v + b * 512:cfg.o_v + (b + 1) * 512], 0)])

            def cons_v(m, ps, b=b):
                st = stb()
                S.cp(alt(), st.v, ps)
                S.dma("sp", self.vloc[m * 128:(m + 1) * 128, b * 512:(b + 1) * 512], st.v)
            gemm_tm(self, wb, 512, KD, act_k, cons_v)

        if ACUT < 5:
            S.barrier_all()
            return
        for c0 in range(0, cfg.DRIN, 512):
            nc_ = min(512, cfg.DRIN - c0)
            wb = ws.load([(W[:, cfg.o_rw + c0:cfg.o_rw + c0 + nc_], 0)])

            def cons_zr(j, w, pss, c0=c0):
                ch = c0 // 128 + j
                for tb in range(NTB):
                    st = stf()
                    S.cp(alt(), st[0:w, 0:TB], pss[tb])
                    S.dma("sp", self.zr[ch * 128:ch * 128 + w, tb * TB:(tb + 1) * TB], st[0:w, 0:TB])
                    if tb == NTB - 1:
                        S.cp("act", tails[0:w, 32 * cfg.NCV + ch:32 * cfg.NCV + ch + 1], st[0:w, TB - 1:TB])
            gemm_fm(self, wb, nc_, KD, act_k, cons_zr)

        if l > 0:
            wb = ws.load([(P['vres_down'][l - 1], 0)])

            def cons_xv(j, w, pss):
                for tb in range(NTB):
                    st = stf()
                    S.cp(alt(), st[0:64, 0:TB], pss[tb])
                    S.dma("sp", self.xvp[0:64, tb * TB:(tb + 1) * TB], st[0:64, 0:TB])
                    if tb == NTB - 1:
                        S.cp("act", tails[0:64, cfg.NTL - 1:cfg.NTL], st[0:64, TB - 1:TB])
            gemm_fm(self, wb, 64, KD, act_k, cons_xv)

        for c0 in range(0, 3 * cfg.D, 512):
            wb = ws.load([(W[:, cfg.o_gate + c0:cfg.o_gate + c0 + 512], 0)])

            def cons_g(j, w, pss, c0=c0):
                ch = c0 // 128 + j
                for tb in range(NTB):
                    st = stf()
                    S.act(st[:, 0:TB], pss[tb], AF.Sigmoid)
                    S.dma("sp", self.gateT[ch // KD][(ch % KD) * 128:(ch % KD + 1) * 128, tb * TB:(tb + 1) * TB], st[:, 0:TB])
            gemm_fm(self, wb, 512, KD, act_k, cons_g)

        S.dma("sp", self.tails_loc.v, tails.v)
        S.barrier_all()
    if not getattr(self, "no_cc", False):
      S.allgather([(self.kloc, self.kall), (self.vloc, self.vall), (self.ikloc, self.ikall),
                 (self.tails_loc, self.tails_all)])

def k_colparams(self, rows, N, name, es):
    S = self.S
    R = len(rows)
    KC = (N + 127) // 128
    out = S.sb(name, [128, KC, R], F32, es=es)
    with ExitStack() as es2:
        raw = S.sb(name + "_raw", [R, N], F32, es=es2)
        for r, ap in enumerate(rows):
            S.dma("sp", raw[r:r + 1, :], V(self.pbuf, ap.rearrange("(o n) -> o n", o=1)))
        for k in range(KC):
            w = min(128, N - k * 128)
            bank = self.psum_bank()
            S.tr(bank[0:w, 0:R], raw[0:R, k * 128:k * 128 + w], self.identf[0:R, 0:R])
            S.cp("act" if k % 2 else "dve", out[0:w, k, :], bank[0:w, 0:R])
        S.barrier_all()
    return out


def k_prevtail(self):
    S, cfg = self.S, self.cfg
    if not hasattr(self, "prevtail"):
        self.prevtail = S.sb("prevtail", [128, cfg.NTL], F32)
    with ExitStack() as es:
        ta = S.sb("tails_ld", [128, 8, cfg.NTL], F32, es=es)
        S.dma("sp", ta.v, self.tails_all.v.r("(r p) c -> p r c", p=128))
        sel = S.sb("selprev", [128, 8], F32, es=es)
        S.dma("sp", sel.v, V(self.pbuf, self.C["selprev"]))
        S.ts("dve", self.prevtail.v, ta[:, 0, :], sel[:, 0:1], None, ALU.mult)
        for r in range(1, 8):
            S.stt("dve", self.prevtail.v, ta[:, r, :], sel[:, r:r + 1], self.prevtail.v, ALU.mult, ALU.add)
        S.barrier_all()


def k_conv(self, l):
    S, cfg, P = self.S, self.cfg, self.P
    TC, TB, NTB, NCV = cfg.TC, cfg.TB, cfg.NTB, cfg.NCV
    with ExitStack() as es:
        import os as _os
        _v = _os.environ.get("CPV", "")
        if _v == "a":
            cp_ = k_colparams(self, [P['dw_bias'][l], P['conv_ln_g'][l], P['conv_ln_b'][l]], cfg.DC, "convp", es)
        elif _v == "b":
            cp_ = k_colparams(self, [P['dw_weight'][l, j] for j in range(31)], cfg.DC, "convp", es)
        else:
            cp_ = k_colparams(self, [P['dw_bias'][l], P['conv_ln_g'][l], P['conv_ln_b'][l]] +
                              [P['dw_weight'][l, j] for j in range(31)], cfg.DC, "convp", es)
        import os as _os
        CUT = int(_os.environ.get("CONVCUT", "9"))
        if CUT < -1:
            S.barrier_all()
            return
        onesf = S.sb("onesf", [128, 128], F32, es=es)
        S.memset("dve", onesf.v, 1.0)
        co = S.sb("co", [128, NCV, TC], F32, es=es)
        sq = S.sb("csq", [128, NCV, TC], F32, es=es)
        cx = [S.sb("cx%d" % i, [128, 32 + TC], F32, es=es) for i in range(2)]
        for j in range(NCV):
            x = cx[j % 2]
            S.dma("sp", x[:, 32:32 + TC], self.cT[j * 128:(j + 1) * 128, :])
            S.cp("act", x[:, 0:32], self.prevtail[:, j * 32:(j + 1) * 32])
            S.ts("dve", co[:, j, :], x[:, 2:2 + TC], cp_[:, j, 3:4], cp_[:, j, 0:1], ALU.mult, ALU.add)
            if CUT < 0:
                continue
            for t in range(1, 31):
                S.stt("dve", co[:, j, :], x[:, 2 + t:2 + t + TC], cp_[:, j, 3 + t:4 + t], co[:, j, :], ALU.mult, ALU.add)
            S.act(sq[:, j, :], co[:, j, :], AF.Square)
        import os as _os
        CUT = int(_os.environ.get("CONVCUT", "9"))
        if CUT < 1:
            S.barrier_all()
            return
        mean = S.sb("cmean", [128, TB], F32, es=es)
        rstd = S.sb("crstd", [128, TB], F32, es=es)
        tmp = S.sb("ctmp", [128, TB], F32, es=es)
        ob = [S.sb("cob%d" % i, [128, TB], BF16, es=es) for i in range(2)]
        for tb in range(NTB):
            ts_ = slice(tb * TB, (tb + 1) * TB)
            p1 = self.psum_bank(); p2 = self.psum_bank()
            for j in range(NCV):
                S.mm(p1[:, 0:TB], onesf.v, co[:, j, ts_], start=(j == 0), stop=(j == NCV - 1))
            for j in range(NCV):
                S.mm(p2[:, 0:TB], onesf.v, sq[:, j, ts_], start=(j == 0), stop=(j == NCV - 1))
            S.ts("dve", mean.v, p1[:, 0:TB], 1.0 / cfg.DC, None, ALU.mult)
            S.tt("dve", tmp.v, mean.v, mean.v, ALU.mult)
            S.stt("dve", rstd.v, p2[:, 0:TB], 1.0 / cfg.DC, tmp.v, ALU.mult, ALU.subtract)
            S.ts("dve", rstd.v, rstd.v, LN_EPS, None, ALU.add)
            S.act(rstd.v, rstd.v, AF.Sqrt)
            S.recip(rstd.v, rstd.v)
            if CUT < 2:
                continue
            for j in range(NCV):
                S.tt("dve", tmp.v, co[:, j, ts_], mean.v, ALU.subtract)
                S.tt("dve", tmp.v, tmp.v, rstd.v, ALU.mult)
                o = ob[j % 2]
                S.act(o.v, tmp.v, AF.Silu, bias=cp_[:, j, 2:3], scale=cp_[:, j, 1:2])
                S.dma("sp", self.apre[j * 128:(j + 1) * 128, ts_], o.v)
        S.barrier_all()


def k_indexer(self, l):
    S, cfg, nc = self.S, self.cfg, self.nc
    TC, L, NT = cfg.TC, cfg.L, cfg.NT
    NSB = L // 512
    NCH = L // 128
    wscale = (16 ** -0.5) * (64 ** -0.5)
    with ExitStack() as es:
        iq = S.sb("iq_sb", [128, 8, TC], BF16, es=es)
        S.dma("sp", iq.v, self.iqT.v.r("(c p) t -> p c t", p=128))
        ik2 = S.sb("ik2", [128, 8, TC], BF16, es=es)
        src = self.ikall.v.r("(r p) t -> p r t", p=64)
        S.dma("sp", ik2[0:64, :, :], src)
        S.dma("sp", ik2[64:128, :, :], src)
        ikf = ik2.v.r("p r t -> p (r t)")
        sidx = S.sb("sidx", [128, L], F32, es=es)
        S.dma("sp", sidx.v, V(self.pbuf, self.C["sidx"].partition_broadcast(128)))
        qpos = S.sb("qpos", [128, NT], F32, es=es)
        S.dma("sp", qpos.v, V(self.pbuf, self.C["qpos"]))
        score = S.sb("score", [128, L], F32, es=es)
        wk = S.sb("tkwork", [128, L], F32, es=es)
        mc = S.sb("mcausal", [128, L], F32, es=es)
        mb = S.sb("mbias", [128, L], BF16, es=es)
        rl = [S.sb("relu%d" % i, [128, 512], F32, es=es) for i in range(3)]
        iwt = S.sb("iwt", [128, 16], F32, es=es)
        m8 = S.sb("m8", [128, 8], F32, es=es)
        mst = [S.sb("mst%d" % i, [128, 8, 128], BF16, es=es) for i in range(2)]
        nr = 0
        for m in range(NT):
            S.dma("sp", iwt.v, self.iw[m * 128:(m + 1) * 128, :])
            S.ts("dve", iwt.v, iwt.v, wscale, None, ALU.mult)
            for sb in range(NSB):
                ssl = slice(sb * 512, (sb + 1) * 512)
                for h in range(16):
                    o = (h % 2) * 64
                    ps = self.psum_bank()
                    S.mm(ps.v, iq[o:o + 64, h // 2, m * 128:(m + 1) * 128], ikf[o:o + 64, ssl])
                    r = rl[nr % 3]; nr += 1
                    S.act(r.v, ps.v, AF.Relu)
                    if h == 0:
                        S.ts("dve", score[:, ssl], r.v, iwt[:, 0:1], None, ALU.mult)
                    else:
                        S.stt("dve", score[:, ssl], r.v, iwt[:, h:h + 1], score[:, ssl], ALU.mult, ALU.add)
            S.ts("dve", mc.v, sidx.v, qpos[:, m:m + 1], None, ALU.is_le)
            S.tt("dve", score.v, score.v, mc.v, ALU.mult)
            S.ts("dve", wk.v, mc.v, 1e30, -1e30, ALU.mult, ALU.add)
            S.tt("dve", score.v, score.v, wk.v, ALU.add)
            cur = score
            nround = cfg.topk // 8
            for it in range(nround):
                S.op("dve", nc.vector.max, [cur.v], [m8.v], out=m8.t[:], in_=cur.t[:])
                if it < nround - 1:
                    S.op("dve", nc.vector.match_replace, [m8.v, cur.v], [wk.v],
                         out=wk.t[:], in_to_replace=m8.t[:], in_values=cur.t[:], imm_value=-3e38)
                    cur = wk
            S.stt("dve", wk.v, score.v, m8[:, 7:8], mc.v, ALU.is_ge, ALU.mult)
            S.ts("dve", mb.v, wk.v, -1.0, -NEG, ALU.add, ALU.mult)
            for c8 in range(NCH // 8):
                bank = self.psum_bank()
                bb = V(bank, bank.t[:].bitcast(BF16))
                for j in range(8):
                    c = c8 * 8 + j
                    S.tr(bb[:, j * 128:(j + 1) * 128], mb[:, c * 128:(c + 1) * 128], self.identb.v)
                st = mst[c8 % 2]
                S.cp("act", st.v.r("p a b -> p (a b)"), bb)
                S.dma("sp", self.maskT[c8 * 8:(c8 + 1) * 8, :, m * 128:(m + 1) * 128].r("c p q -> p c q"), st.v)
        S.barrier_all()


def k_attention(self, l):
    S, cfg = self.S, self.cfg
    TC, L, GQ, NG = cfg.TC, cfg.L, cfg.TB, cfg.NTB
    NCH = L // 128
    scale = 128 ** -0.5
    with ExitStack() as es:
        mk = S.sb("maskg", [128, NCH, GQ], BF16, es=es)
        kT = [S.sb("kTh%d" % i, [128, 8, TC], BF16, es=es) for i in range(2)]
        vh = [S.sb("vh%d" % i, [128, NCH, 128], BF16, es=es) for i in range(2)]
        qh = [S.sb("qh%d" % i, [128, GQ], BF16, es=es) for i in range(2)]
        pT = [S.sb("pT%d" % i, [128, GQ], BF16, es=es) for i in range(3)]
        rs = S.sb("att_rs", [128, GQ], F32, es=es)
        ob = [S.sb("att_ob%d" % i, [128, GQ], BF16, es=es) for i in range(2)]
        kall4 = self.kall.v.r("(r h p) t -> h p r t", h=8, p=128)
        n = 0
        for g in range(NG):
            gs = slice(g * GQ, (g + 1) * GQ)
            S.dma("sp", mk.v, self.maskT[:, :, gs].r("c p q -> p c q"))
            for h in range(8):
                kt = kT[h % 2]; vt = vh[h % 2]; qt = qh[h % 2]
                S.dma("sp", kt.v, kall4[h])
                S.dma("sp", vt.v, self.vall[:, h * 128:(h + 1) * 128].r("(c p) d -> p c d", p=128))
                S.dma("sp", qt.v, self.qT[h * 128:(h + 1) * 128, gs])
                ktf = kt.v.r("p r t -> p (r t)")
                O = self.psum_bank(); Sm = self.psum_bank()
                for c in range(NCH):
                    ps = self.psum_bank()
                    while ps is O or ps is Sm:
                        ps = self.psum_bank()
                    S.mm(ps[:, 0:GQ], ktf[:, c * 128:(c + 1) * 128], qt.v, start=True, stop=False)
                    S.mm(ps[:, 0:GQ], self.identb.v, mk[:, c, :], start=False, stop=True)
                    p = pT[n % 3]; n += 1
                    S.act(p.v, ps[:, 0:GQ], AF.Exp, scale=scale)
                    S.mm(O[:, 0:GQ], vt[:, c, :], p.v, start=(c == 0), stop=(c == NCH - 1))
                    S.mm(Sm[:, 0:GQ], self.onesb.v, p.v, start=(c == 0), stop=(c == NCH - 1))
                S.recip(rs.v, Sm[:, 0:GQ])
                o = ob[h % 2]
                S.tt("dve", o.v, O[:, 0:GQ], rs.v, ALU.mult)
                S.dma("sp", self.bpre[h * 128:(h + 1) * 128, gs], o.v)
        S.barrier_all()

def k_alloc_scratch2(self):
    cfg = self.cfg
    sc = self.scratch
    TC = cfg.TC
    self.apre = sc("apre", [cfg.DC, TC], BF16)
    self.bpre = sc("bpre", [1024, TC], BF16)
    self.cpre = sc("cpre", [cfg.DR, TC], BF16)
    self.maskT = sc("maskT", [cfg.L // 128, 128, TC], BF16)
    self.mergedT = sc("mergedT", [cfg.D, TC], BF16)
    self.vfirst = sc("vfirst", [cfg.DR, TC], F32)


def k_merge(self, l):
    S, cfg, P = self.S, self.cfg, self.P
    TC, TB, NTB, D = cfg.TC, cfg.TB, cfg.NTB, cfg.D
    with ExitStack() as es:
        ops = []
        for (nm, src, kc) in (("a", self.apre, cfg.NCV), ("b", self.bpre, 8), ("c", self.cpre, cfg.NP)):
            t = S.sb("op_" + nm, [128, kc, TC], BF16, es=es)
            S.dma("sp", t.v, src.v.r("(k p) t -> p k t", p=128))
            ops.append((t, kc))
        BW = 256
        wss = [WStream(self, kc, es, width=BW, name="wm%d" % i) for i, (t, kc) in enumerate(ops)]
        Ws = [P['w_conv_out'][l], P['w_att_out'][l], P['w_rwkv_out'][l]]
        gst = [S.sb("gst%d" % i, [128, TB], F32, es=es) for i in range(4)]
        acc = [S.sb("macc%d" % i, [128, TB], F32, es=es) for i in range(2)]
        mo = [S.sb("mo%d" % i, [128, TB], BF16, es=es) for i in range(2)]
        ng = 0
        for c0 in range(0, D, BW):
            wbs = [wss[i].load([(Ws[i][:, c0:c0 + BW], 0)]) for i in range(3)]
            for j in range(BW // 128):
                ch = c0 // 128 + j
                pss = []
                for i in range(3):
                    t, kc = ops[i]
                    banks = [self.psum_bank() for _ in range(NTB)]
                    for k in range(kc):
                        for tb in range(NTB):
                            S.mm(banks[tb][:, 0:TB], wbs[i][:, k, j * 128:(j + 1) * 128],
                                 t[:, k, tb * TB:(tb + 1) * TB], start=(k == 0), stop=(k == kc - 1))
                    pss.append(banks)
                for tb in range(NTB):
                    ts_ = slice(tb * TB, (tb + 1) * TB)
                    a = acc[tb % 2]
                    for i in range(3):
                        g = gst[ng % 4]; ng += 1
                        S.dma("sp", g.v, self.gateT_src[i][ch * 128:(ch + 1) * 128, ts_])
                        if i == 0:
                            S.tt("dve", a.v, g.v, pss[i][tb][:, 0:TB], ALU.mult)
                        else:
                            S.tt("dve", g.v, g.v, pss[i][tb][:, 0:TB], ALU.mult)
                            if i == 1:
                                S.tt("dve", a.v, a.v, g.v, ALU.add)
                            else:
                                o = mo[tb % 2]
                                S.tt("dve", o.v, a.v, g.v, ALU.add)
                                S.dma("sp", self.mergedT[ch * 128:(ch + 1) * 128, ts_], o.v)
        S.barrier_all()


def x_update_consumer(self, c0, xst, cnt, src=None):
    src = src or self.xT
    S, cfg = self.S, self.cfg
    TB, NTB = cfg.TB, cfg.NTB

    def cons(j, w, pss):
        ch = c0 // 128 + j
        for tb in range(NTB):
            ts_ = slice(tb * TB, (tb + 1) * TB)
            x = xst[cnt[0] % len(xst)]; cnt[0] += 1
            S.dma("sp", x.v, src[ch * 128:(ch + 1) * 128, ts_])
            S.tt("dve", x.v, x.v, pss[tb], ALU.add)
            S.dma("sp", self.xT[ch * 128:(ch + 1) * 128, ts_], x.v)
    return cons


def k_wout(self, l):
    S, cfg, P = self.S, self.cfg, self.P
    with ExitStack() as es:
        mt = S.sb("mergsb", [128, cfg.KD, cfg.TC], BF16, es=es)
        S.dma("sp", mt.v, self.mergedT.v.r("(k p) t -> p k t", p=128))
        ws = WStream(self, cfg.KD, es)
        xst = [S.sb("xst%d" % i, [128, cfg.TB], F32, es=es) for i in range(4)]
        cnt = [0]
        for c0 in range(0, cfg.D, 512):
            wb = ws.load([(P['w_out'][l][:, c0:c0 + 512], 0)])
            gemm_fm(self, wb, 512, cfg.KD, lambda k: mt[:, k, :], x_update_consumer(self, c0, xst, cnt, src=self.xT_src))
        S.barrier_all()


def k_ffn(self, l):
    S, cfg, P = self.S, self.cfg, self.P
    TC, TB, NTB, KD, KF = cfg.TC, cfg.TB, cfg.NTB, cfg.KD, cfg.KF
    PMAX = 22
    parts = []
    c = 0
    while c < KF:
        n = min(PMAX, KF - c)
        parts.append((c, n)); c += n
    with ExitStack() as es:
        hT = S.sb("hTf", [128, KD, TC], BF16, es=es)
        k_norm(self, P['norm_ffn'][l], hT, es)
        act = S.sb("ffact", [128, PMAX, TC], BF16, es=es)
        ws = WStream(self, max(KD, PMAX), es)
        sgs = [S.sb("sg%d" % i, [128, TB], F32, es=es) for i in range(3)]
        xst = [S.sb("xsf%d" % i, [128, TB], F32, es=es) for i in range(4)]
        cnt = [0]; ns = [0]
        Wg, Wu, Wd = P['w_ffn_gate'][l], P['w_ffn_up'][l], P['w_ffn_down'][l]
        for (p0, pn) in parts:
            for b in range(0, pn, 2):
                f0 = (p0 + b) * 128
                ws.KC = KD
                wb = ws.load([(Wg[:, f0:f0 + 256], 0), (Wu[:, f0:f0 + 256], 256)])
                held = {}
                gemm_fm(self, wb, 256, KD, lambda k: hT[:, k, :], lambda j, w, pss: held.__setitem__(j, pss), col0=0)

                def cons_up(j, w, pss, b=b, held=held):
                    for tb in range(NTB):
                        sg = sgs[ns[0] % 3]; ns[0] += 1
                        S.act(sg.v, held[j][tb], AF.Silu)
                        S.tt("dve", act[:, b + j, tb * TB:(tb + 1) * TB], sg.v, pss[tb], ALU.mult)
                gemm_fm(self, wb, 256, KD, lambda k: hT[:, k, :], cons_up, col0=256)
            for c0 in range(0, cfg.D, 512):
                ws.KC = pn
                wb = ws.load([(Wd[p0 * 128:(p0 + pn) * 128, c0:c0 + 512], 0)])
                gemm_fm(self, wb, 512, pn, lambda k: act[:, k, :], x_update_consumer(self, c0, xst, cnt))
        S.barrier_all()


def k_final(self):
    S, cfg, P = self.S, self.cfg, self.P
    TC, TB, NTB, KD = cfg.TC, cfg.TB, cfg.NTB, cfg.KD
    with ExitStack() as es:
        gcol = k_colparams(self, [P['norm_final']], cfg.D, "gfin", es)
        xblk = S.sb("xblkf", [128, KD, TB], F32, es=es)
        sq = [S.sb("sqf%d" % i, [128, TB], BF16, es=es) for i in range(2)]
        rstd = S.sb("rstdf", [128, TB], F32, es=es)
        ost = [S.sb("ostf%d" % i, [128, 512], F32, es=es) for i in range(2)]
        no = 0
        for tb in range(NTB):
            t0 = tb * TB
            accb = self.psum_bank()
            for k in range(KD):
                S.dma("sp", xblk[:, k, :], self.xT[k * 128:(k + 1) * 128, t0:t0 + TB])
                s = sq[k % 2]
                S.act(s.v, xblk[:, k, :], AF.Square)
                S.mm(accb[:, 0:TB], self.onesb.v, s.v, start=(k == 0), stop=(k == KD - 1))
            S.ts("dve", rstd.v, accb[:, 0:TB], 1.0 / cfg.D, RMS_EPS, ALU.mult, ALU.add)
            S.act(rstd.v, rstd.v, AF.Sqrt)
            S.recip(rstd.v, rstd.v)
            for k in range(KD):
                S.stt("dve", xblk[:, k, :], xblk[:, k, :], gcol[:, k, 0:1], rstd.v, ALU.mult, ALU.mult)
            for tt_ in range(TB // 128):
                for k4 in range(KD // 4):
                    bank = self.psum_bank()
                    for j in range(4):
                        S.tr(bank[:, j * 128:(j + 1) * 128], xblk[:, k4 * 4 + j, tt_ * 128:(tt_ + 1) * 128], self.identf.v)
                    o = ost[no % 2]; no += 1
                    S.cp("act" if no % 2 else "dve", o.v, bank.v)
                    S.dma("sp", self.out[t0 + tt_ * 128:t0 + (tt_ + 1) * 128, k4 * 512:(k4 + 1) * 512], o.v)
        S.barrier_all()

DECAY_K = float(np.exp(-0.5))


def k_alloc_scratch3(self):
    cfg = self.cfg
    sc = self.scratch
    TC, NP = cfg.TC, cfg.NP
    NCK = TC // 64
    self.NCK = NCK
    self.rw_m64 = sc("rw_m64", [NP, NCK, 64, 6 * 64], BF16)
    self.rw_tok = sc("rw_tok", [NP, NCK, 64, 3 * 128], BF16)
    self.rw_feat = sc("rw_feat", [NP, 128, 2 * TC], BF16)
    self.rw_gam = sc("rw_gam", [NP, 128, NCK], F32)
    self.rw_g = sc("rw_g", [cfg.DR, TC], F32)
    self.rw_bonus = sc("rw_bonus", [cfg.DR, TC], F32)
    self.st_loc = sc("st_loc", [NP * 128, 256], F32)
    self.st_all = sc("st_all", [8 * NP * 128, 256], F32)
    self.rw_y = sc("rw_y", [cfg.DR, TC], F32)
    self.h_init = sc("h_init", [NP * 128, 128], F32)


def k_rwkv_prep(self, l):
    S, cfg, P, nc = self.S, self.cfg, self.P, self.nc
    TC, TB, NTB, NT, NP, DR = cfg.TC, cfg.TB, cfg.NTB, cfg.NT, cfg.NP, cfg.DR
    NCK = self.NCK
    tb0 = 32 * cfg.NCV
    mu = P['rwkv_mu'][l]
    with ExitStack() as es:
        bm = S.sb("bmask", [128, 4, 128], F32, es=es)
        S.dma("sp", bm.v, V(self.pbuf, self.C["bmask"].rearrange("a p t -> p a t")))
        rst = S.sb("scanrst", [128, TC], F32, es=es)
        S.dma("sp", rst.v, V(self.pbuf, self.C["scanrst"]))
        bonesf = bm[:, 3, :]
        def shifted(rows0, n, name, pieces, dt=BF16, func=None):
            z = S.sb(name + "_z", [n, 1 + TC], F32, es=es)
            S.dma("sp", z[:, 1:1 + TC], self.zr[rows0:rows0 + n, :])
            for (psl, col, d0) in pieces:
                cnt_ = psl.stop - psl.start
                S.cp("act", z[d0:d0 + cnt_, 0:1], self.prevtail[psl, col:col + 1])
            mcol = k_colparams(self, [mu[rows0:rows0 + n]], n, name + "_mu", es)
            d = S.sb(name + "_d", [n, TC], F32, es=es)
            S.tt("dve", d.v, z[:, 0:TC], z[:, 1:1 + TC], ALU.subtract)
            S.stt("dve", d.v, d.v, mcol[0:n, 0, 0:1], z[:, 1:1 + TC], ALU.mult, ALU.add)
            o = S.sb(name + "_s", [n, TC], dt, es=es)
            if func is None:
                S.cp("act", o.v, d.v)
            else:
                S.act(o.v, d.v, func)
            return o
        c3 = 3 * NP
        xw = shifted(3 * DR, 96, "xw", [(slice(0, 96), tb0 + c3, 0)], func=AF.Tanh)
        xa = shifted(3 * DR + 96, 96, "xa", [(slice(96, 128), tb0 + c3, 0), (slice(0, 32), tb0 + c3 + 1, 32), (slice(32, 64), tb0 + c3 + 1, 64)])
        xg0 = shifted(3 * DR + 192, 128, "xg0", [(slice(64, 128), tb0 + c3 + 1, 0), (slice(0, 64), tb0 + c3 + 2, 64)],
                      func=AF.Sigmoid)
        xg1 = shifted(3 * DR + 320, 128, "xg1", [(slice(64, 128), tb0 + c3 + 2, 0), (slice(0, 64), tb0 + c3 + 3, 64)],
                      func=AF.Sigmoid)
        w2b = S.sb("w2b", [96, DR], BF16, es=es)
        a2b = S.sb("a2b", [96, DR], BF16, es=es)
        g2b = S.sb("g2b", [128, 2, DR], BF16, es=es)
        vupb = S.sb("vupb", [64, DR], BF16, es=es)
        with ExitStack() as es3:
            wf = S.sb("w2f", [96, DR], F32, es=es3)
            S.dma("sp", wf.v, V(self.pbuf, P['rwkv_w2'][l]))
            S.cp("act", w2b.v, wf.v)
            af = S.sb("a2f", [96, DR], F32, es=es3)
            S.dma("sp", af.v, V(self.pbuf, P['rwkv_a2'][l]))
            S.cp("dve", a2b.v, af.v)
            gf = S.sb("g2f", [128, 2, DR], F32, es=es3)
            S.dma("sp", gf.v, V(self.pbuf, P['rwkv_g2'][l].rearrange("(k p) c -> p k c", p=128)))
            S.cp("act", g2b.v, gf.v)
            if l > 0:
                vf = S.sb("vupf", [64, DR], F32, es=es3)
                S.dma("sp", vf.v, V(self.pbuf, P['vres_up'][l - 1]))
                S.cp("dve", vupb.v, vf.v)
            S.barrier_all()
        if l > 0:
            xvz = S.sb("xvz", [64, 1 + TC], F32, es=es)
            S.dma("sp", xvz[:, 1:1 + TC], self.xvp.v)
            S.cp("act", xvz[:, 0:1], self.prevtail[0:64, cfg.NTL - 1:cfg.NTL])
            vmu = k_colparams(self, [P['vres_mu'][l - 1]], 64, "vmu", es)
            xvd = S.sb("xvd", [64, TC], F32, es=es)
            S.tt("dve", xvd.v, xvz[:, 0:TC], xvz[:, 1:1 + TC], ALU.subtract)
            S.stt("dve", xvd.v, xvd.v, vmu[0:64, 0, 0:1], xvz[:, 1:1 + TC], ALU.mult, ALU.add)
            xvb = S.sb("xvb", [64, TC], BF16, es=es)
            S.cp("act", xvb.v, xvd.v)
        pr = [P['rwkv_w0'][l], P['rwkv_a0'][l], P['rwkv_k_k'][l], P['rwkv_k_a'][l],
              P['rwkv_r_k'][l].rearrange("h n -> (h n)"), mu[0:DR], mu[DR:2 * DR], mu[2 * DR:3 * DR]]
        if l > 0:
            pr.append(P['vres_bias'][l - 1])
        cp_ = k_colparams(self, pr, DR, "rwp", es)
        W0, A0, KK, KA, RK, MR, MK, MV, VB = range(9)

        def f32t(name, w=TC):
            return S.sb(name, [128, w], F32, es=es)
        zr_ = [f32t("zr_r", 1 + TC), f32t("zr_k", 1 + TC), f32t("zr_v", 1 + TC)]
        r_s, k_s, v_s, lw, alr, gt, kk, cum, t1, t2, E = [f32t(n) for n in
                                                          ("r_s", "k_s", "v_s", "lw", "alr", "gt", "kk", "cum", "t1", "t2", "E")]
        bvec = f32t("bvec"); k2 = f32t("k2")
        bft = {n: S.sb("b_" + n, [128, TC], BF16, es=es) for n in ("rt", "at", "bt", "kt", "bh", "kh", "vb")}
        gam = S.sb("gam", [128, NCK], F32, es=es)
        Nb = S.sb("Nb", [128, 2, 128], BF16, es=es)
        Nb2 = S.sb("Nb2", [128, 2, 128], BF16, es=es)
        Mf = S.sb("Mf", [128, 128], F32, es=es)
        Mb = S.sb("Mb", [128, 128], BF16, es=es)
        Aak = S.sb("Aak", [128, 128], BF16, es=es)
        atT = S.sb("atT", [128, 128], BF16, es=es)
        m64 = S.sb("m64", [64, 2, 6 * 64], BF16, es=es)
        tok = S.sb("tok", [64, 2, 3 * 128], BF16, es=es)
        WT = S.sb("WTp", [128, TC], BF16, es=es)

        for p in range(NP):
            for i, (zt, out, mi) in enumerate(((zr_[0], r_s, MR), (zr_[1], k_s, MK), (zr_[2], v_s, MV))):
                ch = i * NP + p
                S.dma("sp", zt[:, 1:1 + TC], self.zr[ch * 128:(ch + 1) * 128, :])
                S.cp("act", zt[:, 0:1], self.prevtail[:, tb0 + ch:tb0 + ch + 1])
                S.tt("dve", out.v, zt[:, 0:TC], zt[:, 1:1 + TC], ALU.subtract)
                S.stt("dve", out.v, out.v, cp_[:, p, mi:mi + 1], zt[:, 1:1 + TC], ALU.mult, ALU.add)
            pc = slice(p * 128, (p + 1) * 128)
            for tb in range(NTB):
                ts_ = slice(tb * TB, (tb + 1) * TB)
                ps = self.psum_bank()
                S.mm(ps[:, 0:TB], w2b[:, pc], xw[:, ts_])
                S.act(lw[:, ts_], ps[:, 0:TB], AF.Sigmoid, bias=cp_[:, p, W0:W0 + 1])
                ps = self.psum_bank()
                S.mm(ps[:, 0:TB], a2b[:, pc], xa[:, ts_])
                S.act(alr[:, ts_], ps[:, 0:TB], AF.Sigmoid, bias=cp_[:, p, A0:A0 + 1])
                ps = self.psum_bank()
                S.mm(ps[:, 0:TB], g2b[:, 0, pc], xg0[:, ts_], start=True, stop=False)
                S.mm(ps[:, 0:TB], g2b[:, 1, pc], xg1[:, ts_], start=False, stop=True)
                S.cp("act", gt[:, ts_], ps[:, 0:TB])
                if l > 0:
                    ps = self.psum_bank()
                    S.mm(ps[:, 0:TB], vupb[:, pc], xvb[:, ts_])
                    S.act(t1[:, ts_], ps[:, 0:TB], AF.Sigmoid, bias=cp_[:, p, VB:VB + 1])
            S.dma("sp", self.rw_g[pc, :], gt.v)
            if l > 0:
                S.dma("sp", t2.v, self.vfirst[pc, :])
                S.tt("dve", t2.v, t2.v, v_s.v, ALU.subtract)
                S.tt("dve", t2.v, t2.v, t1.v, ALU.mult)
                S.tt("dve", v_s.v, v_s.v, t2.v, ALU.add)
            else:
                S.dma("sp", self.vfirst[pc, :], v_s.v)
            S.ts("dve", lw.v, lw.v, -DECAY_K, None, ALU.mult)
            S.ts("dve", kk.v, k_s.v, cp_[:, p, KK:KK + 1], None, ALU.mult)
            S.act(t1.v, kk.v, AF.Square)
            for tb in range(NTB):
                ts_ = slice(tb * TB, (tb + 1) * TB)
                ps = self.psum_bank()
                S.mm(ps[:, 0:TB], bonesf, t1[:, ts_])
                S.act(t2[:, ts_], ps[:, 0:TB], AF.Sqrt)
            S.ts("dve", t2.v, t2.v, 1e-12, None, ALU.max)
            S.recip(t2.v, t2.v)
            S.tt("dve", kk.v, kk.v, t2.v, ALU.mult)
            S.ts("dve", t1.v, alr.v, -1.0, cp_[:, p, KA:KA + 1], ALU.add, ALU.mult)
            S.stt("dve", k2.v, t1.v, 1.0, k_s.v, ALU.add, ALU.mult)
            S.tt("dve", bvec.v, kk.v, alr.v, ALU.mult)
            S.stt("dve", t1.v, r_s.v, cp_[:, p, RK:RK + 1], k2.v, ALU.mult, ALU.mult)
            for tb in range(NTB):
                ts_ = slice(tb * TB, (tb + 1) * TB)
                ps = self.psum_bank()
                S.mm(ps[:, 0:TB], bonesf, t1[:, ts_])
                S.tt("dve", t2[:, ts_], ps[:, 0:TB], v_s[:, ts_], ALU.mult)
            S.dma("sp", self.rw_bonus[pc, :], t2.v)
            S.op("dve", nc.vector.tensor_tensor_scan, [rst.v, lw.v], [cum.v],
                 cum.t[:], rst.t[:], lw.t[:], 0.0, ALU.mult, ALU.add)
            cum3 = cum.v.r("p (c t) -> p c t", t=64)
            cumC = V(cum, cum3.ap[:, :, 63:64].to_broadcast([128, NCK, 64]))
            S.act(E.v, cum.v, AF.Exp)
            S.tt("dve", t1.v, r_s.v, E.v, ALU.mult)
            S.cp("act", bft["rt"].v, t1.v)
            S.cp("dve", gam.v.r("p (c o) -> p c o", o=1), E.v.r("p (c t) -> p c t", t=64)[:, :, 63:64])
            S.dma("sp", self.rw_gam[p], gam.v)
            S.act(E.v, cum.v, AF.Exp, scale=-1.0)
            S.tt("dve", t1.v, bvec.v, E.v, ALU.mult)
            S.cp("act", bft["bt"].v, t1.v)
            S.tt("dve", t1.v, k2.v, E.v, ALU.mult)
            S.cp("act", bft["kt"].v, t1.v)
            S.tt("dve", t1.v, cum.v, lw.v, ALU.subtract)
            S.act(E.v, t1.v, AF.Exp)
            S.stt("dve", t1.v, kk.v, -1.0, E.v, ALU.mult, ALU.mult)
            S.cp("act", bft["at"].v, t1.v)
            S.stt("dve", t1.v.r("p (c t) -> p c t", t=64), cum3, -1.0, cumC, ALU.mult, ALU.add)
            S.act(E.v, t1.v, AF.Exp)
            S.tt("dve", t1.v, bvec.v, E.v, ALU.mult)
            S.cp("act", bft["bh"].v, t1.v)
            S.tt("dve", t1.v, k2.v, E.v, ALU.mult)
            S.cp("act", bft["kh"].v, t1.v)
            S.cp("act", bft["vb"].v, v_s.v)
            for m in range(NT):
                tsl = slice(m * 128, (m + 1) * 128)
                bank = self.psum_bank()
                bb = V(bank, bank.t[:].bitcast(BF16))
                S.tr(bb[:, 0:128], bft["at"][:, tsl], self.identb.v)
                S.cp("act", atT.v, bb[:, 0:128])
                bank = self.psum_bank()
                bb = V(bank, bank.t[:].bitcast(BF16))
                for q in range(2):
                    for i, nm in enumerate(("bh", "kh", "vb")):
                        c0 = (q * 3 + i) * 128
                        S.tr(bb[0:64, c0:c0 + 128], bft[nm][:, m * 128 + q * 64:m * 128 + (q + 1) * 64], self.identb.v)
                S.cp("act", tok.v.r("s q c -> s (q c)"), bb[0:64, 0:768])
                for e in range(2):
                    o = 64 * e
                    hs = slice(o, o + 64)
                    at_, bt_, kt_, rt_ = (bft[n][hs, tsl] for n in ("at", "bt", "kt", "rt"))
                    bank = self.psum_bank()
                    S.mm(bank[:, 0:128], bt_, at_)
                    S.mm(bank[:, 128:256], at_, bt_)
                    S.mm(bank[:, 256:384], at_, kt_)
                    S.tt("dve", Nb[:, 0, :], bank[:, 0:128], bm[:, 0, :], ALU.mult)
                    S.tt("dve", Nb[:, 1, :], bank[:, 128:256], bm[:, 1, :], ALU.mult)
                    S.tt("dve", Aak.v, bank[:, 256:384], bm[:, 1, :], ALU.mult)
                    S.tt("dve", Mf.v, bank[:, 0:128], bm[:, 0, :], ALU.mult)
                    S.tt("dve", Mf.v, Mf.v, self.identf.v, ALU.add)
                    S.cp("act", Mb.v, Mf.v)
                    bank2 = self.psum_bank()
                    for q in range(2):
                        qs = slice(m * 128 + q * 64, m * 128 + (q + 1) * 64)
                        S.mm(bank2[0:64, (2 * q) * 64:(2 * q + 1) * 64], bft["bt"][hs, qs], bft["rt"][hs, qs])
                        S.mm(bank2[0:64, (2 * q + 1) * 64:(2 * q + 2) * 64], bft["kt"][hs, qs], bft["rt"][hs, qs])
                    for q in range(2):
                        S.tt("dve", m64[:, q, (2 + e) * 64:(3 + e) * 64], bank2[0:64, (2 * q) * 64:(2 * q + 1) * 64],
                             bm[0:64, 2, 0:64], ALU.mult)
                        S.tt("dve", m64[:, q, (4 + e) * 64:(5 + e) * 64], bank2[0:64, (2 * q + 1) * 64:(2 * q + 2) * 64],
                             bm[0:64, 2, 0:64], ALU.mult)
                    cur, nxt = Nb, Nb2
                    for i in range(1, 6):
                        bk = self.psum_bank()
                        S.mm(bk[:, 128:256], cur[:, 0, :], cur[:, 1, :])
                        if i < 5:
                            S.mm(bk[:, 0:128], cur[:, 1, :], cur[:, 0, :])
                            S.cp("act", nxt.v.r("p a b -> p (a b)"), bk[:, 0:256])
                        else:
                            S.cp("act", nxt[:, 1, :], bk[:, 128:256])
                        bk2 = self.psum_bank()
                        S.mm(bk2[:, 0:128], nxt[:, 1, :], Mb.v)
                        S.tt("dve", Mf.v, Mf.v, bk2[:, 0:128], ALU.add)
                        S.cp("act", Mb.v, Mf.v)
                        cur, nxt = nxt, cur
                    bk = self.psum_bank()
                    if e == 0:
                        S.mm(bk[0:64, 0:128], atT[:, 0:64], Mb.v)
                        S.cp("act", WT[0:64, tsl], bk[0:64, 0:128])
                    else:
                        S.mm(bk[:, 0:128], atT[:, 0:128], Mb.v)
                        S.cp("act", WT[64:128, tsl], bk[64:128, 0:128])
                    bk = self.psum_bank()
                    for q in range(2):
                        S.mm(bk[0:64, q * 64:(q + 1) * 64], Aak[:, q * 64:(q + 1) * 64], Mb[:, q * 64:(q + 1) * 64])
                    for q in range(2):
                        S.cp("act", m64[:, q, e * 64:(e + 1) * 64], bk[0:64, q * 64:(q + 1) * 64])
                S.dma("sp", self.rw_m64[p, 2 * m:2 * m + 2].r("q s c -> s q c"), m64.v)
                S.dma("sp", self.rw_tok[p, 2 * m:2 * m + 2].r("q s c -> s q c"), tok.v)
            S.dma("sp", self.rw_feat[p, :, 0:TC], WT.v)
            S.dma("sp", self.rw_feat[p, :, TC:2 * TC], bft["rt"].v)
        S.barrier_all()

def k_rwkv_scan(self, l, pass2):
    S, cfg = self.S, self.cfg
    TC, NP = cfg.TC, cfg.NP
    NCK = self.NCK
    WID = 128 if pass2 else 256
    with ExitStack() as es:
        feat = S.sb("feat", [128, NP, 2 * TC], BF16, es=es)
        S.dma("sp", feat.v, self.rw_feat.v.r("n p c -> p n c"))
        gam = S.sb("gamr", [128, NP, NCK], F32, es=es)
        S.dma("sp", gam.v, self.rw_gam.v.r("n p c -> p n c"))
        H = S.sb("Hst", [128, NP, WID], F32, es=es)
        Hb = S.sb("Hbf", [128, NP, WID], BF16, es=es)
        if pass2:
            S.dma("sp", H.v, self.h_init.v.r("(n p) c -> p n c", p=128))
            y = S.sb("ysb", [128, NP, TC], F32, es=es)
            S.memset("dve", y.v, 0.0)
        else:
            S.memset("dve", H.v, 0.0)
            for p in range(NP):
                S.cp("dve", H[:, p, 128:256], self.identf.v)
        S.cp("act", Hb.v.r("p n c -> p (n c)"), H.v.r("p n c -> p (n c)"))
        NB = 4
        m64 = [S.sb("m64r%d" % i, [64, 6 * 64], BF16, es=es) for i in range(NB)]
        tok = [S.sb("tokr%d" % i, [64, 3 * 128], BF16, es=es) for i in range(NB)]
        Ub = [S.sb("Ub%d" % i, [64, WID], BF16, es=es) for i in range(NB)]
        if pass2:
            UE = [S.sb("UE%d" % i, [64, 2, 128], BF16, es=es) for i in range(NB)]
            VE = [S.sb("VE%d" % i, [64, 2, 128], BF16, es=es) for i in range(NB)]
            for t in UE + VE:
                S.memset("dve", t.v, 0.0)
        n = 0
        import os as _os
        P2CUT = int(_os.environ.get("P2CUT", "9")) if pass2 else 9
        for c in range(NCK if P2CUT > 0 else 0):
            cs = slice(c * 64, (c + 1) * 64)
            for p in range(NP):
                i = n % NB; n += 1
                S.dma("sp", m64[i].v, self.rw_m64[p, c])
                S.dma("sp", tok[i].v, self.rw_tok[p, c])
                bh = tok[i][:, 0:128]; kh = tok[i][:, 128:256]; vT = tok[i][:, 256:384]
                ups = self.psum_bank()
                S.mm(ups[0:64, 0:WID], feat[:, p, cs], Hb[:, p, :], start=True, stop=False)
                for e in range(2):
                    S.mm(ups[0:64, e * 64:(e + 1) * 64], m64[i][:, e * 64:(e + 1) * 64], vT[:, e * 64:(e + 1) * 64],
                         start=False, stop=(e == 1))
                S.cp("act", Ub[i].v, ups[0:64, 0:WID])
                if pass2 and P2CUT >= 2:
                    for e in range(2):
                        S.cp("dve", UE[i][:, e, e * 64:(e + 1) * 64], Ub[i][:, e * 64:(e + 1) * 64])
                        S.cp("dve", VE[i][:, e, e * 64:(e + 1) * 64], vT[:, e * 64:(e + 1) * 64])
                if pass2 and P2CUT >= 3:
                    yps = self.psum_bank()
                    S.mm(yps[:, 0:64], Hb[:, p, :], feat[:, p, TC + c * 64:TC + (c + 1) * 64], start=True, stop=False)
                    for e in range(2):
                        S.mm(yps[:, 0:64], UE[i][:, e, :], m64[i][:, (2 + e) * 64:(3 + e) * 64], start=False, stop=False)
                        S.mm(yps[:, 0:64], VE[i][:, e, :], m64[i][:, (4 + e) * 64:(5 + e) * 64], start=False, stop=(e == 1))
                    S.cp("act", y[:, p, cs], yps[:, 0:64])
                hps = self.psum_bank()
                S.mm(hps[:, 0:WID], bh, Ub[i].v, start=True, stop=False)
                S.mm(hps[:, 0:128], kh, vT, start=False, stop=True)
                for e in range(2):
                    o = 64 * e
                    for base in ((0,) if pass2 else (0, 128)):
                        S.stt("dve", H[o:o + 64, p, base + o:base + o + 64], H[o:o + 64, p, base + o:base + o + 64],
                              gam[o:o + 64, p, c:c + 1], hps[o:o + 64, base + o:base + o + 64], ALU.mult, ALU.add)
                S.cp("act", Hb[:, p, :], H[:, p, :])
        if pass2:
            S.dma("sp", self.rw_y.v.r("(n p) t -> p n t", p=128), y.v)
        else:
            S.dma("sp", self.st_loc.v.r("(n p) c -> p n c", p=128), H.v)
        S.barrier_all()


def k_rwkv_compose(self, l):
    S, cfg = self.S, self.cfg
    NP = cfg.NP
    with ExitStack() as es:
        sel = S.sb("selpre", [128, 8], F32, es=es)
        S.dma("sp", sel.v, V(self.pbuf, self.C["selpre"]))
        sta = [S.sb("sta%d" % i, [128, 8, 256], F32, es=es) for i in range(2)]
        Pc = S.sb("Pc", [128, 128], F32, es=es)
        Tt = S.sb("Tt", [128, 128], F32, es=es)
        hi = [S.sb("hi%d" % i, [128, 128], F32, es=es) for i in range(2)]
        stall = self.st_all.v.r("(r n p) c -> n p r c", n=NP, p=128)
        for p in range(NP):
            st = sta[p % 2]
            S.dma("sp", st.v, stall[p])
            h = hi[p % 2]
            S.memset("dve", Pc.v, 0.0)
            S.memset("dve", h.v, 0.0)
            for r in range(7):
                bk = self.psum_bank()
                S.tr(bk[:, 0:128], st[:, r, 128:256], self.identf.v)
                S.cp("act", Tt.v, bk[:, 0:128])
                bk2 = self.psum_bank()
                S.mm(bk2[:, 0:128], Tt.v, Pc.v)
                S.tt("dve", Pc.v, bk2[:, 0:128], st[:, r, 0:128], ALU.add)
                S.stt("dve", h.v, Pc.v, sel[:, r + 1:r + 2], h.v, ALU.mult, ALU.add)
            S.dma("sp", self.h_init[p * 128:(p + 1) * 128, :], h.v)
        S.barrier_all()


def k_rwkv_post(self, l):
    S, cfg, P = self.S, self.cfg, self.P
    TC, TB, NTB, NP, DR = cfg.TC, cfg.TB, cfg.NTB, cfg.NP, cfg.DR
    with ExitStack() as es:
        cp_ = k_colparams(self, [P['rwkv_ln_g'][l], P['rwkv_ln_b'][l]], DR, "rwpost", es)
        bm = S.sb("bones", [128, 128], F32, es=es)
        S.dma("sp", bm.v, V(self.pbuf, self.C["bmask"][3]))
        yt = [S.sb("yt%d" % i, [128, TC], F32, es=es) for i in range(2)]
        sq = S.sb("ysq", [128, TC], F32, es=es)
        mean = S.sb("ymean", [128, TC], F32, es=es)
        rstd = S.sb("yrstd", [128, TC], F32, es=es)
        bo = [S.sb("ybo%d" % i, [128, TC], F32, es=es) for i in range(2)]
        gg = [S.sb("ygg%d" % i, [128, TC], F32, es=es) for i in range(2)]
        ob = [S.sb("yob%d" % i, [128, TC], BF16, es=es) for i in range(2)]
        for p in range(NP):
            pc = slice(p * 128, (p + 1) * 128)
            y = yt[p % 2]; b = bo[p % 2]; g = gg[p % 2]; o = ob[p % 2]
            S.dma("sp", y.v, self.rw_y[pc, :])
            S.dma("sp", b.v, self.rw_bonus[pc, :])
            S.dma("sp", g.v, self.rw_g[pc, :])
            S.act(sq.v, y.v, AF.Square)
            for tb in range(NTB):
                ts_ = slice(tb * TB, (tb + 1) * TB)
                p1 = self.psum_bank(); p2 = self.psum_bank()
                S.mm(p1[:, 0:TB], bm.v, y[:, ts_])
                S.mm(p2[:, 0:TB], bm.v, sq[:, ts_])
                S.ts("dve", mean[:, ts_], p1[:, 0:TB], 1.0 / 64, None, ALU.mult)
                S.ts("dve", rstd[:, ts_], p2[:, 0:TB], 1.0 / 64, None, ALU.mult)
            S.tt("dve", sq.v, mean.v, mean.v, ALU.mult)
            S.tt("dve", rstd.v, rstd.v, sq.v, ALU.subtract)
            S.ts("dve", rstd.v, rstd.v, GN_EPS, None, ALU.add)
            S.act(rstd.v, rstd.v, AF.Sqrt)
            S.recip(rstd.v, rstd.v)
            S.tt("dve", y.v, y.v, mean.v, ALU.subtract)
            S.tt("dve", y.v, y.v, rstd.v, ALU.mult)
            S.ts("dve", y.v, y.v, cp_[:, p, 0:1], cp_[:, p, 1:2], ALU.mult, ALU.add)
            S.tt("dve", y.v, y.v, b.v, ALU.add)
            S.tt("dve", o.v, y.v, g.v, ALU.mult)
            S.dma("sp", self.cpre[pc, :], o.v)
        S.barrier_all()


def k_rwkv(self, l):
    import os as _os
    cut = int(_os.environ.get("RWCUT", "9"))
    k_rwkv_prep(self, l)
    if cut < 2: return
    k_rwkv_scan(self, l, False)
    if cut < 3: return
    if _os.environ.get("AG2K"):
        self.S.allgather([(self.kloc, self.kall)])
    elif not _os.environ.get("NOAG2"):
        self.S.allgather([(self.st_loc, self.st_all)])
    if not _os.environ.get("NOCOMP"):
        k_rwkv_compose(self, l)
    if cut < 4: return
    k_rwkv_scan(self, l, True)
    if cut < 5: return
    k_rwkv_post(self, l)

def k_build_all(self, layers=None, final=True):
    cfg = self.cfg
    k_setup(self)
    k_alloc_scratch(self); k_alloc_scratch2(self); k_alloc_scratch3(self)
    k_load_x(self)
    for l in (range(cfg.depth) if layers is None else layers):
        k_phaseA(self, l)
        k_prevtail(self)
        k_conv(self, l)
        k_indexer(self, l)
        k_attention(self, l)
        k_rwkv(self, l)
        k_merge(self, l)
        k_wout(self, l)
        k_ffn(self, l)
    if final:
        k_final(self)
    self.flush_dbg()


A_PARAMS = ['w_in', 'norm_mix']
B_PARAMS = ['dw_weight', 'dw_bias', 'conv_ln_g', 'conv_ln_b', 'rwkv_mu', 'rwkv_w0', 'rwkv_w2', 'rwkv_a0', 'rwkv_a2',
            'rwkv_g2', 'rwkv_k_k', 'rwkv_k_a', 'rwkv_r_k']
C_PARAMS = ['rwkv_ln_g', 'rwkv_ln_b', 'w_conv_out', 'w_att_out', 'w_rwkv_out', 'w_out', 'norm_ffn',
            'w_ffn_gate', 'w_ffn_up', 'w_ffn_down']
A_OUT = ['cT', 'qT', 'kloc', 'vloc', 'iqT', 'ikloc', 'iw', 'zr', 'gateT0', 'gateT1', 'gateT2', 'tails_loc']
B_IN = ['cT', 'qT', 'kall', 'vall', 'iqT', 'ikall', 'iw', 'zr', 'tails_all']
B_OUT = ['apre', 'bpre', 'st_loc', 'rw_m64', 'rw_tok', 'rw_feat', 'rw_gam', 'rw_g', 'rw_bonus']
C_IN = ['st_all', 'rw_m64', 'rw_tok', 'rw_feat', 'rw_gam', 'rw_g', 'rw_bonus', 'apre', 'bpre', 'gateT_in0', 'gateT_in1', 'gateT_in2', 'xT_in']


def _a_spec(l):
    p = {n: l for n in A_PARAMS}
    outs = list(A_OUT)
    if l > 0:
        p['vres_down'] = l - 1
        outs.append('xvp')
    return p, outs


def build_launch(cfg, kind, l):
    last = (l == cfg.depth - 1)
    if kind == 'A':
        params, outs = _a_spec(l)
        kb = K(cfg, ext_out=outs + ['xT'], params=params, use_x=True, use_out=False)
    elif kind == 'B':
        params = {n: l for n in B_PARAMS}
        ins = list(B_IN); outs = list(B_OUT)
        if l > 0:
            for n in ('vres_mu', 'vres_up', 'vres_bias'):
                params[n] = l - 1
            ins += ['xvp', 'vfirst']
        else:
            outs.append('vfirst')
        kb = K(cfg, ext_in=ins, ext_out=outs, params=params, use_x=False, use_out=False)
    else:
        params = {n: l for n in C_PARAMS}
        outs = []
        if last:
            params['norm_final'] = None
        else:
            pa, oa = _a_spec(l + 1)
            params.update(pa)
            outs = oa + ['xT']
        kb = K(cfg, ext_in=C_IN, ext_out=outs, params=params, use_x=False, use_out=last)
    kb.no_cc = True
    with kb.es:
        k_setup(kb)
        k_alloc_scratch(kb); k_alloc_scratch2(kb); k_alloc_scratch3(kb)
        if kind == 'A':
            k_load_x(kb)
            k_phaseA(kb, l)
        elif kind == 'B':
            k_prevtail(kb)
            k_conv(kb, l)
            k_indexer(kb, l)
            k_attention(kb, l)
            k_rwkv_prep(kb, l)
            k_rwkv_scan(kb, l, False)
        else:
            import os as _os
            cstop = int(_os.environ.get("CSTOP", "99"))
            steps = [lambda: k_rwkv_compose(kb, l), lambda: k_rwkv_scan(kb, l, True), lambda: k_rwkv_post(kb, l),
                     lambda: k_merge(kb, l), lambda: k_wout(kb, l), lambda: k_ffn(kb, l),
                     (lambda: k_final(kb)) if last else (lambda: k_phaseA(kb, l + 1))]
            for i, st_ in enumerate(steps):
                if i < cstop:
                    st_()
        kb.S.barrier_all()
    return kb


_CACHE = {}
_CONSTS = {}


def _launch(cfg, kind, l, per_core, inputs):
    key = (cfg.D, cfg.L, kind, l)
    if key not in _CACHE:
        _CACHE[key] = build_launch(cfg, kind, l)
    kb = _CACHE[key]
    ckey = (cfg.D, cfg.L)
    if ckey not in _CONSTS:
        _CONSTS[ckey] = [make_consts(cfg, c) for c in range(8)]
    in_maps = []
    pvals = {}
    for n, li in kb.params.items():
        a = np.asarray(inputs[n], dtype=np.float32)
        a = a if n == 'norm_final' else a[li]
        if n in kb.split_info:
            for i, (c0, c1) in enumerate(kb.split_info[n]):
                pvals["%s__%d" % (n, i)] = np.ascontiguousarray(a[:, c0:c1])
        else:
            pvals[n] = np.ascontiguousarray(a)
    for c in range(8):
        m = dict(per_core[c])
        m.update(pvals)
        for n, v in _CONSTS[ckey][c].items():
            m["c_" + n] = v
        in_maps.append(m)
    res = run_bass_kernel_spmd(kb.nc, in_maps, core_ids=list(range(8)))
    return res.results


def _gather(results, name):
    return np.concatenate([np.asarray(r[name]) for r in results], axis=0)


def kernel(**inputs):
    x = np.asarray(inputs['x'], dtype=np.float32)
    B, L, D = x.shape
    cfg = Cfg(D, L)
    TC = cfg.TC
    per_core = [{"x": np.ascontiguousarray(x[0, c * TC:(c + 1) * TC])} for c in range(8)]
    ra = _launch(cfg, 'A', 0, per_core, inputs)
    vfirst = None
    out = None
    for l in range(cfg.depth):
        kall = _gather(ra, 'kloc'); vall = _gather(ra, 'vloc'); ikall = _gather(ra, 'ikloc')
        tails_all = _gather(ra, 'tails_loc')
        per_core = []
        for c in range(8):
            m = {n: ra[c][n] for n in ('cT', 'qT', 'iqT', 'iw', 'zr')}
            m.update(kall=kall, vall=vall, ikall=ikall, tails_all=tails_all)
            if l > 0:
                m['xvp'] = ra[c]['xvp']
                m['vfirst'] = vfirst[c]
            per_core.append(m)
        rb = _launch(cfg, 'B', l, per_core, inputs)
        if l == 0:
            vfirst = [rb[c]['vfirst'] for c in range(8)]
        st_all = _gather(rb, 'st_loc')
        per_core = []
        for c in range(8):
            m = {n: rb[c][n] for n in ('rw_m64', 'rw_tok', 'rw_feat', 'rw_gam', 'rw_g', 'rw_bonus', 'apre', 'bpre')}
            m['st_all'] = st_all
            for i in range(3):
                m['gateT_in%d' % i] = ra[c]['gateT%d' % i]
            m['xT_in'] = ra[c]['xT']
            per_core.append(m)
        rc = _launch(cfg, 'C', l, per_core, inputs)
        if l == cfg.depth - 1:
            out = np.concatenate([np.asarray(r["out"], dtype=np.float32) for r in rc], axis=0)
        ra = rc
    return out.reshape(B, L, D)
```
